# Optimizing a Trainium2 kernel written in Bass

```python
import math
import jax, jax.numpy as jnp
from jax import lax
import numpy as np

D_MODEL = 2048
BATCH = 1
SEQ = 8192
DEPTH = 1
DEC_BATCH = 128
DEC_SEQ = 4
PAST_LEN = 2048
PAGE_SIZE = 128

MIX_WIDTH = D_MODEL
ATTN_WIDTH = MIX_WIDTH // 2
POOL_WIDTH = MIX_WIDTH - ATTN_WIDTH
HEAD_DIM = 128
N_HEADS = ATTN_WIDTH // HEAD_DIM
POOL_WINDOWS = (2, 4, 8, 16)
N_POOL_GROUPS = len(POOL_WINDOWS)
POOL_GROUP_W = POOL_WIDTH // N_POOL_GROUPS
POOL_STATE = max(POOL_WINDOWS) - 1
MOBA_BLOCK = 256
MOBA_TOPK = 3
Q_BLOCK = 128
N_EXPERT_GROUPS = 4
EXPERTS_PER_GROUP = 4
N_EXPERTS = N_EXPERT_GROUPS * EXPERTS_PER_GROUP
EXPERT_TOPK = 2
D_EXPERT = 3 * D_MODEL // 8
EPS = 1e-6
NEG = -1e30

kernel_name = "hymba_moba_pool_hmoe_step"


def rmsnorm(x, g):
    xf = x.astype(jnp.float32)
    y = xf * lax.rsqrt(jnp.mean(xf * xf, axis=-1, keepdims=True) + EPS)
    return (y * g.astype(jnp.float32)).astype(x.dtype)


def to_bh(t):
    return jnp.transpose(t, (0, 2, 1, 3))


def pad_blocks(t):
    L = t.shape[2]
    Lp = -(-L // MOBA_BLOCK) * MOBA_BLOCK
    return jnp.pad(t, ((0, 0), (0, 0), (0, Lp - L), (0, 0)))


def block_means(k):
    B, H, Lp, D = k.shape
    return jnp.mean(k.reshape(B, H, Lp // MOBA_BLOCK, MOBA_BLOCK, D).astype(jnp.float32), axis=3)


def mixer_inputs(x, g_mix, w_in, g_q, g_k):
    B, S, _ = x.shape
    h = rmsnorm(x, g_mix)
    z = jnp.einsum('bsd,de->bse', h, w_in)
    q = z[..., :ATTN_WIDTH].reshape(B, S, N_HEADS, HEAD_DIM)
    k = z[..., ATTN_WIDTH:2 * ATTN_WIDTH].reshape(B, S, N_HEADS, HEAD_DIM)
    v = z[..., 2 * ATTN_WIDTH:3 * ATTN_WIDTH].reshape(B, S, N_HEADS, HEAD_DIM)
    u = z[..., 3 * ATTN_WIDTH:]
    return rmsnorm(q, g_q), rmsnorm(k, g_k), v, u


def moba_query_block(q, q0, k, v, kmean):
    B, H, T, D = q.shape
    nb = kmean.shape[2]
    topk = min(MOBA_TOPK, nb)
    own = q0 // MOBA_BLOCK
    qf = q.astype(jnp.float32)
    gate = jnp.einsum('bhtd,bhnd->bhtn', qf, kmean)
    gate = jnp.where(jnp.arange(nb) < own, gate, NEG)
    _, sel = lax.top_k(gate, topk)
    sel_ok = sel < own
    bi = jnp.arange(B)[:, None, None, None]
    hi = jnp.arange(H)[None, :, None, None]
    kb = k.reshape(B, H, nb, MOBA_BLOCK, D)
    vb = v.reshape(B, H, nb, MOBA_BLOCK, D)
    k_sel = kb[bi, hi, sel].astype(jnp.float32)
    v_sel = vb[bi, hi, sel].astype(jnp.float32)
    k_own = lax.dynamic_slice_in_dim(k, own * MOBA_BLOCK, MOBA_BLOCK, axis=2).astype(jnp.float32)
    v_own = lax.dynamic_slice_in_dim(v, own * MOBA_BLOCK, MOBA_BLOCK, axis=2).astype(jnp.float32)
    scale = HEAD_DIM ** -0.5
    s_sel = jnp.einsum('bhtd,bhtnkd->bhtnk', qf, k_sel) * scale
    s_sel = jnp.where(sel_ok[..., None], s_sel, NEG)
    s_own = jnp.einsum('bhtd,bhkd->bhtk', qf, k_own) * scale
    kpos = own * MOBA_BLOCK + jnp.arange(MOBA_BLOCK)
    qpos = q0 + jnp.arange(T)
    s_own = jnp.where(kpos[None, :] <= qpos[:, None], s_own, NEG)
    s = jnp.concatenate([s_sel.reshape(B, H, T, topk * MOBA_BLOCK), s_own], axis=-1)
    p = jax.nn.softmax(s, axis=-1)
    p_sel = p[..., :topk * MOBA_BLOCK].reshape(B, H, T, topk, MOBA_BLOCK)
    p_own = p[..., topk * MOBA_BLOCK:]
    o = jnp.einsum('bhtnk,bhtnkd->bhtd', p_sel, v_sel) + jnp.einsum('bhtk,bhkd->bhtd', p_own, v_own)
    return o.astype(q.dtype)


def moba_prompt(q, k, v):
    B, H, S, D = q.shape
    kp = pad_blocks(k)
    vp = pad_blocks(v)
    kmean = block_means(kp)

    def one(c):
        q0 = c * Q_BLOCK
        qc = lax.dynamic_slice_in_dim(q, q0, Q_BLOCK, axis=2)
        return moba_query_block(qc, q0, kp, vp, kmean)

    o = lax.map(one, jnp.arange(S // Q_BLOCK))
    return jnp.transpose(o, (1, 2, 0, 3, 4)).reshape(B, H, S, D)


def pool_mix(u, prev, start_pos, w_pool, pool_scale):
    B, S, P = u.shape
    ext = jnp.concatenate([prev.astype(u.dtype), u], axis=1).astype(jnp.float32)
    cs = jnp.concatenate([jnp.zeros((B, 1, P), jnp.float32), jnp.cumsum(ext, axis=1)], axis=1)
    pos = (start_pos + jnp.arange(S)).astype(jnp.float32)
    uf = u.astype(jnp.float32)
    end0 = POOL_STATE + 1
    outs = []
    for g, w in enumerate(POOL_WINDOWS):
        c0, c1 = g * POOL_GROUP_W, (g + 1) * POOL_GROUP_W
        wsum = cs[:, end0:end0 + S, c0:c1] - cs[:, end0 - w:end0 - w + S, c0:c1]
        count = jnp.minimum(jnp.float32(w), pos + 1.0)[None, :, None]
        d = wsum / count - uf[..., c0:c1]
        outs.append(jnp.einsum('bsc,ce->bse', d, w_pool[g].astype(jnp.float32)))
    y = jnp.concatenate(outs, axis=-1) * pool_scale.astype(jnp.float32)
    return y.astype(u.dtype), ext[:, -POOL_STATE:].astype(u.dtype)


def mixer_output(x, o_attn, o_pool, w_out):
    B, S, _ = x.shape
    mix = jnp.concatenate([o_attn.reshape(B, S, ATTN_WIDTH).astype(x.dtype), o_pool.astype(x.dtype)], axis=-1)
    return x + jnp.einsum('bse,ed->bsd', mix, w_out)


def hier_moe(h, w_gr, b_gr, w_er, b_er, w_gate, w_up, w_down):
    N = h.shape[0]
    hf = h.astype(jnp.float32)
    g_logits = hf @ w_gr.astype(jnp.float32) + b_gr.astype(jnp.float32)
    g_prob = jax.nn.softmax(g_logits, axis=-1)
    g_top = jnp.argmax(g_logits, axis=-1)
    g_p = jnp.take_along_axis(g_prob, g_top[:, None], axis=-1)
    e_logits = jnp.einsum('nd,dge->nge', hf, w_er.astype(jnp.float32)) + b_er.astype(jnp.float32)
    e_logits = jnp.take_along_axis(e_logits, g_top[:, None, None], axis=1)[:, 0]
    e_val, e_idx = lax.top_k(e_logits, EXPERT_TOPK)
    e_w = jax.nn.softmax(e_val, axis=-1) * g_p
    expert_id = g_top[:, None] * EXPERTS_PER_GROUP + e_idx
    combine = jnp.sum(jax.nn.one_hot(expert_id, N_EXPERTS, dtype=jnp.float32) * e_w[..., None], axis=1)
    a = jnp.einsum('nd,edf->nef', h, w_gate)
    b = jnp.einsum('nd,edf->nef', h, w_up)
    act = jax.nn.silu(a) * b * combine[..., None].astype(h.dtype)
    return jnp.einsum('nef,efd->nd', act, w_down)


def ffn_block(x, g_ffn, w_gr, b_gr, w_er, b_er, w_gate, w_up, w_down):
    h = rmsnorm(x, g_ffn).reshape(-1, x.shape[-1])
    return x + hier_moe(h, w_gr, b_gr, w_er, b_er, w_gate, w_up, w_down).reshape(x.shape).astype(x.dtype)


def setup_inputs(seed: int = 0) -> dict:
    key = jax.random.key(seed)
    ks = jax.random.split(key, 24)
    f32 = jnp.float32
    n_pages = PAST_LEN // PAGE_SIZE
    n_phys = (5 * DEC_BATCH * n_pages + 3) // 4
    L = DEPTH
    mix_in = 3 * ATTN_WIDTH + POOL_WIDTH

    def nrm(k, shape, scale=1.0):
        return jax.random.normal(k, shape, f32) * scale

    def gain(k, shape):
        return 1.0 + 0.02 * jax.random.normal(k, shape, f32)

    perm = jax.random.permutation(ks[5], n_phys)[:DEC_BATCH * n_pages]
    return {
        "x_prompt": nrm(ks[0], (BATCH, SEQ, D_MODEL)),
        "x_sample": nrm(ks[1], (DEC_BATCH, DEC_SEQ, D_MODEL)),
        "cache_k": nrm(ks[2], (L, n_phys, PAGE_SIZE, N_HEADS, HEAD_DIM)),
        "cache_v": nrm(ks[3], (L, n_phys, PAGE_SIZE, N_HEADS, HEAD_DIM)),
        "state_pool": nrm(ks[4], (L, DEC_BATCH, POOL_STATE, POOL_WIDTH)),
        "page_table": perm.reshape(DEC_BATCH, n_pages).astype(jnp.int32),
        "g_mix": gain(ks[6], (L, D_MODEL)),
        "w_in": nrm(ks[7], (L, D_MODEL, mix_in), D_MODEL ** -0.5),
        "g_q": gain(ks[8], (L, HEAD_DIM)),
        "g_k": gain(ks[9], (L, HEAD_DIM)),
        "w_pool": nrm(ks[10], (L, N_POOL_GROUPS, POOL_GROUP_W, POOL_GROUP_W), POOL_GROUP_W ** -0.5),
        "pool_scale": 1.0 + 0.1 * nrm(ks[11], (L, POOL_WIDTH)),
        "w_out": nrm(ks[12], (L, MIX_WIDTH, D_MODEL), MIX_WIDTH ** -0.5),
        "g_ffn": gain(ks[13], (L, D_MODEL)),
        "w_group_router": nrm(ks[14], (L, D_MODEL, N_EXPERT_GROUPS), D_MODEL ** -0.5),
        "b_group_router": nrm(ks[15], (L, N_EXPERT_GROUPS), 0.01),
        "w_expert_router": nrm(ks[16], (L, D_MODEL, N_EXPERT_GROUPS, EXPERTS_PER_GROUP), D_MODEL ** -0.5),
        "b_expert_router": nrm(ks[17], (L, N_EXPERT_GROUPS, EXPERTS_PER_GROUP), 0.01),
        "w_gate": nrm(ks[18], (L, N_EXPERTS, D_MODEL, D_EXPERT), D_MODEL ** -0.5),
        "w_up": nrm(ks[19], (L, N_EXPERTS, D_MODEL, D_EXPERT), D_MODEL ** -0.5),
        "w_down": nrm(ks[20], (L, N_EXPERTS, D_EXPERT, D_MODEL), D_EXPERT ** -0.5),
    }


def reference(x_prompt, x_sample, cache_k, cache_v, state_pool, page_table, g_mix, w_in, g_q, g_k,
              w_pool, pool_scale, w_out, g_ffn, w_group_router, b_group_router, w_expert_router,
              b_expert_router, w_gate, w_up, w_down):
    B, S, _ = x_prompt.shape
    DB, T, _ = x_sample.shape
    n_pages = page_table.shape[1]
    past_len = n_pages * cache_k.shape[2]
    h_p, h_s = x_prompt, x_sample
    kp_l, vp_l, pp_l, ks_l, vs_l, ps_l = [], [], [], [], [], []
    for l in range(DEPTH):
        q, k, v, u = mixer_inputs(h_p, g_mix[l], w_in[l], g_q[l], g_k[l])
        o_attn = jnp.transpose(moba_prompt(to_bh(q), to_bh(k), to_bh(v)), (0, 2, 1, 3))
        o_pool, pool_new = pool_mix(u, jnp.zeros((B, POOL_STATE, POOL_WIDTH), u.dtype), 0, w_pool[l], pool_scale[l])
        h_p = mixer_output(h_p, o_attn, o_pool, w_out[l])
        h_p = ffn_block(h_p, g_ffn[l], w_group_router[l], b_group_router[l], w_expert_router[l],
                        b_expert_router[l], w_gate[l], w_up[l], w_down[l])
        kp_l.append(k); vp_l.append(v); pp_l.append(pool_new)
        q, k, v, u = mixer_inputs(h_s, g_mix[l], w_in[l], g_q[l], g_k[l])
        k_past = cache_k[l][page_table].reshape(DB, past_len, N_HEADS, HEAD_DIM)
        v_past = cache_v[l][page_table].reshape(DB, past_len, N_HEADS, HEAD_DIM)
        k_all = pad_blocks(to_bh(jnp.concatenate([k_past, k.astype(k_past.dtype)], axis=1)))
        v_all = pad_blocks(to_bh(jnp.concatenate([v_past, v.astype(v_past.dtype)], axis=1)))
        o_attn = moba_query_block(to_bh(q), past_len, k_all, v_all, block_means(k_all))
        o_attn = jnp.transpose(o_attn, (0, 2, 1, 3))
        o_pool, pool_new = pool_mix(u, state_pool[l], past_len, w_pool[l], pool_scale[l])
        h_s = mixer_output(h_s, o_attn, o_pool, w_out[l])
        h_s = ffn_block(h_s, g_ffn[l], w_group_router[l], b_group_router[l], w_expert_router[l],
                        b_expert_router[l], w_gate[l], w_up[l], w_down[l])
        ks_l.append(k); vs_l.append(v); ps_l.append(pool_new)
    y_prompt = h_p
    y_sample = h_s
    k_prompt = jnp.stack(kp_l)
    v_prompt = jnp.stack(vp_l)
    pool_prompt = jnp.stack(pp_l)
    k_sample = jnp.stack(ks_l)
    v_sample = jnp.stack(vs_l)
    pool_sample = jnp.stack(ps_l)
    return (y_prompt, y_sample, k_prompt, v_prompt, pool_prompt, k_sample, v_sample, pool_sample)
```

```python
import numpy as np
from contextlib import ExitStack
import concourse.bass as bass
import concourse.mybir as mybir
from concourse.bass_utils import run_bass_kernel_spmd

F32 = mybir.dt.float32
BF16 = mybir.dt.bfloat16
I32 = mybir.dt.int32
ALU = mybir.AluOpType
AF = mybir.ActivationFunctionType
AX = mybir.AxisListType

D = 2048
S = 8192
NSEQ = 128
TS = 4
NS = NSEQ * TS
NTOK = S + NS
NT = NTOK // 128
HD = 128
NH = 8
NPAGES = 16
NPHYS = 2560
EPS = 1e-6
SCALE = HD ** -0.5
NEG = -1.0e30


class _St:
    def __init__(self):
        self.w = None
        self.r = {}


class TB:
    def __init__(self, a, name, st=None):
        self.a = a
        self.name = name
        self.s = st if st is not None else _St()
        self.dsem = None
        self.dcnt = 0

    @property
    def w(self):
        return self.s.w

    @w.setter
    def w(self, v):
        self.s.w = v

    @property
    def r(self):
        return self.s.r

    @r.setter
    def r(self, v):
        self.s.r = v

    def __getitem__(self, idx):
        return self.a[idx]


class Tag:
    __slots__ = ("key", "sem", "val", "buf")

    def __init__(self, key, sem, val, buf=None):
        self.key, self.sem, self.val, self.buf = key, sem, val, buf


class KB:
    def __init__(self, nc, es):
        self.nc, self.es = nc, es
        self.E = {"pe": nc.tensor, "act": nc.scalar, "dve": nc.vector, "pool": nc.gpsimd, "sp": nc.sync}
        self.sem = {k: es.enter_context(nc.semaphore("sem_" + k)) for k in self.E}
        self.cnt = {k: 0 for k in self.E}
        self.seen = {k: {} for k in self.E}
        self.outs = []
        self.n = 0

    def sb(self, name, shape, dt):
        return TB(self.es.enter_context(self.nc.sbuf_tensor(name, shape, dt)), name)

    def banks(self):
        self.bk = [self.es.enter_context(self.nc.psum_tensor("bank%d" % i, [128, 512], F32)) for i in range(8)]
        self.bst = [_St() for i in range(8)]

    def ps(self, name, bank, off, n, dt=F32, split=None, parts=128):
        a = self.bk[bank][0:parts, off:off + n]
        if dt == BF16:
            a = a.bitcast(BF16)
        if split is not None:
            a = a.rearrange("p (a b) -> p a b", a=split)
        return TB(a, name, self.bst[bank])

    def dr(self, name, shape, dt, kind):
        return TB(self.nc.dram_tensor(name, shape, dt, kind=kind).ap(), name)

    def _wait(self, e, reads, writes):
        tags = []
        for b in reads:
            if b.w is not None:
                tags.append(b.w)
        for b in writes:
            if b.w is not None:
                tags.append(b.w)
            tags.extend(b.r.values())
        eng = self.E[e]
        for t in tags:
            if e == "pe" and t.key == "pe":
                continue
            val = t.buf.dcnt if t.buf is not None else t.val
            if self.seen[e].get(t.key, 0) < val:
                eng.wait_ge(t.sem, val)
                self.seen[e][t.key] = val

    def op(self, e, fn, reads=(), writes=()):
        self._wait(e, reads, writes)
        ins = fn(self.E[e])
        self.cnt[e] += 1
        ins.then_inc(self.sem[e], 1)
        tag = Tag(e, self.sem[e], self.cnt[e])
        for b in reads:
            b.r[e] = tag
        for b in writes:
            b.w = tag
            b.r = {}
        return ins

    def _dma_done(self, ins, reads, writes, final):
        b = writes[0] if writes else reads[0]
        if b.dsem is None:
            self.n += 1
            b.dsem = self.es.enter_context(self.nc.semaphore("dsem%d" % self.n))
        b.dcnt += 16
        ins.then_inc(b.dsem, 16)
        tag = Tag("d_" + b.name, b.dsem, b.dcnt, b)
        for x in reads:
            x.r[tag.key] = tag
        for x in writes:
            x.w = tag
            x.r = {}
        if final and b not in self.outs:
            self.outs.append(b)

    def dma(self, q, out, in_, reads=(), writes=(), final=False):
        self._wait(q, reads, writes)
        ins = self.E[q].dma_start(out=out, in_=in_)
        self._dma_done(ins, list(reads), list(writes), final)

    def gather(self, out, in_, idx_ap, reads=(), writes=()):
        self._wait("pool", reads, writes)
        ins = self.nc.gpsimd.indirect_dma_start(
            out=out, out_offset=None, in_=in_,
            in_offset=bass.IndirectOffsetOnAxis(ap=idx_ap, axis=0))
        self._dma_done(ins, list(reads), list(writes), False)

    def finish(self):
        for b in self.outs:
            self.nc.sync.wait_ge(b.dsem, b.dcnt)
        for e in self.E:
            if e != "sp" and self.cnt[e] > 0:
                self.nc.sync.wait_ge(self.sem[e], self.cnt[e])


def _consts(kb):
    nc = kb.nc
    onesf = kb.sb("onesf", [128, 128], F32)
    identf = kb.sb("identf", [128, 128], F32)
    identb = kb.sb("identb", [128, 128], BF16)
    trib = kb.sb("trib", [128, 128], BF16)
    kb.op("pool", lambda e: e.memset(onesf[:], 1.0), writes=[onesf])
    kb.op("pool", lambda e: e.affine_select(out=identf[:], in_=onesf[:], pattern=[[1, 128]],
                                            compare_op=ALU.is_equal, fill=0.0, base=0,
                                            channel_multiplier=-1), reads=[onesf], writes=[identf])
    kb.op("pool", lambda e: e.tensor_copy(out=identb[:], in_=identf[:]), reads=[identf], writes=[identb])
    kb.op("pool", lambda e: e.affine_select(out=trib[:], in_=onesf[:], pattern=[[1, 128]],
                                            compare_op=ALU.is_ge, fill=0.0, base=0,
                                            channel_multiplier=-1), reads=[onesf], writes=[trib])
    return onesf, identf, identb, trib


def build_attn(nphys=NPHYS, ngroups=16, tiles=None, nchunks=64, nseq=NSEQ, xrows=NTOK, stop=0, sub=9):
    nc = bass.Bass("TRN2", target_bir_lowering=False)
    es = ExitStack()
    with es:
        kb = KB(nc, es)
        kb.banks()
        x = kb.dr("x", [xrows, D], F32, "ExternalInput")
        wh = kb.dr("wh", [D, 512], F32, "ExternalInput")
        gmix = kb.dr("gmix", [1, D], F32, "ExternalInput")
        gq = kb.dr("gq", [1, HD], F32, "ExternalInput")
        gk = kb.dr("gk", [1, HD], F32, "ExternalInput")
        kc = kb.dr("kc", [nphys, HD * 128], F32, "ExternalInput")
        vc = kb.dr("vc", [nphys, 128 * HD], F32, "ExternalInput")
        pt = kb.dr("pt", [NSEQ * NPAGES, 1], I32, "ExternalInput")
        kvu = kb.dr("kvu", [NTOK, 3, HD], F32, "ExternalOutput")
        oo = kb.dr("oo", [NTOK, HD], F32, "ExternalOutput")
        scrK = [kb.dr("scrK%d" % g, [128, HD * 128], F32, "Internal") for g in range(16)]
        scrV = [kb.dr("scrV%d" % g, [128, HD * 128], F32, "Internal") for g in range(16)]
        kvu_s = TB(kvu.a, "kvu_s")

        onesf, identf, identb, trib = _consts(kb)

        ptt = kb.sb("ptt", [128, 16], I32)
        ptt2 = kb.sb("ptt2", [128, 16, 2], I32)
        stage = kb.sb("stage", [128, HD * 64], F32)
        for g in range(16):
            kb.dma("sp", ptt[:, g:g + 1], pt.a[g * 128:(g + 1) * 128, :], writes=[ptt])
        kb.op("dve", lambda e: e.tensor_scalar(out=ptt2[:, :, 0], in0=ptt[:], scalar1=2.0, scalar2=None,
                                               op0=ALU.mult), reads=[ptt], writes=[ptt2])
        kb.op("dve", lambda e: e.tensor_scalar(out=ptt2[:, :, 1], in0=ptt[:], scalar1=2.0, scalar2=1.0,
                                               op0=ALU.mult, op1=ALU.add), reads=[ptt], writes=[ptt2])
        kc2 = kc.a.rearrange("n (h e) -> (n h) e", h=2)
        vc2 = vc.a.rearrange("n (h e) -> (n h) e", h=2)
        for g in range(ngroups):
            for (src, scr) in ((kc2, scrK[g]), (vc2, scrV[g])):
                for hf in range(2):
                    kb.gather(stage[:], src, ptt2[:, g, hf:hf + 1], reads=[ptt2], writes=[stage])
                    kb.dma("sp", scr.a[:, hf * HD * 64:(hf + 1) * HD * 64], stage[:], reads=[stage], writes=[scr])

        gbc = kb.sb("gbc", [128, D], F32)
        gqb = kb.sb("gqb", [128, HD], F32)
        gkb = kb.sb("gkb", [128, HD], F32)
        wb = kb.sb("wb", [128, 16, 512], BF16)
        kb.dma("sp", gbc[:], gmix.a[0, :].partition_broadcast(128), writes=[gbc])
        kb.dma("sp", gqb[:], gq.a[0, :].partition_broadcast(128), writes=[gqb])
        kb.dma("sp", gkb[:], gk.a[0, :].partition_broadcast(128), writes=[gkb])
        kb.dma("pool", wb[:], wh.a.rearrange("(c p) n -> p c n", p=128), writes=[wb])

        if stop == 1:
            kb.dma("sp", oo.a[0:128, :], onesf[:], reads=[onesf], final=True)
            kb.finish()
            return nc
        QT = kb.sb("QT", [128, NTOK], BF16)
        KT = kb.sb("KT", [128, NTOK], BF16)
        Vx = kb.sb("Vx", [128, NT, HD + 1], BF16)
        ksum = kb.sb("ksum", [128, 64], F32)
        kmT = kb.sb("kmT", [128, 32], BF16)
        kb.op("pool", lambda e: e.memset(Vx[:, :, HD:HD + 1], 1.0), writes=[Vx])
        kb.op("pool", lambda e: e.memset(ksum[:], 0.0), writes=[ksum])

        xt = [kb.sb("xt%d" % i, [128, D], F32) for i in range(2)]
        junk = kb.sb("junk", [128, D], BF16)
        hn = kb.sb("hn", [128, D], BF16)
        hT = [kb.sb("hT%d" % i, [128, 16, 128], BF16) for i in range(2)]
        st = [kb.sb("st%d" % i, [128, 8], F32) for i in range(2)]
        o3 = [kb.sb("o3%d" % i, [128, 3, HD], F32) for i in range(2)]
        qn = [kb.sb("qn%d" % i, [128, 2, HD], BF16) for i in range(2)]
        zp = [kb.ps("zp%d" % i, i, 0, 512) for i in range(2)]
        tp = [kb.ps("tp%d" % i, 2 + i, 0, 512, BF16, split=8) for i in range(2)]
        tq = kb.ps("tq", 4, 0, 128, BF16, split=2)

        for i in (range(NT) if tiles is None else tiles):
            X, Hh, Sx, O3, Qn, Z = xt[i % 2], hT[i % 2], st[i % 2], o3[i % 2], qn[i % 2], zp[i % 2]
            kb.dma("sp", X[:], x.a[i * 128:(i + 1) * 128, :], writes=[X])
            kb.op("act", lambda e: e.activation(out=junk[:], in_=X[:], func=AF.Square,
                                                accum_out=Sx[:, 0:1]), reads=[X], writes=[junk, Sx])
            kb.op("act", lambda e: e.activation(out=Sx[:, 1:2], in_=Sx[:, 0:1], func=AF.Sqrt,
                                                scale=1.0 / D, bias=EPS), reads=[Sx], writes=[Sx])
            kb.op("dve", lambda e: e.reciprocal(out=Sx[:, 2:3], in_=Sx[:, 1:2]), reads=[Sx], writes=[Sx])
            kb.op("dve", lambda e: e.scalar_tensor_tensor(out=hn[:], in0=X[:], scalar=Sx[:, 2:3], in1=gbc[:],
                                                          op0=ALU.mult, op1=ALU.mult),
                  reads=[X, Sx, gbc], writes=[hn])
            if stop == 2:
                kb.dma("sp", oo.a[0:128, :], onesf[:], reads=[onesf], final=True); kb.finish(); return nc
            for half in range(2):
                T = tp[half]
                for cc in range(8):
                    c = half * 8 + cc
                    kb.op("pe", lambda e: e.transpose(out=T[:, cc, :], in_=hn[:, c * 128:(c + 1) * 128],
                                                      identity=identb[:]), reads=[hn, identb], writes=[T])
                if half == 0:
                    kb.op("act", lambda e: e.copy(out=Hh[:, 0:8, :], in_=T[:]), reads=[T], writes=[Hh])
                else:
                    kb.op("dve", lambda e: e.tensor_copy(out=Hh[:, 8:16, :], in_=T[:]), reads=[T], writes=[Hh])
            if stop == 3:
                kb.dma("sp", oo.a[0:128, :], onesf[:], reads=[onesf], final=True); kb.finish(); return nc
            for c in range(16):
                kb.op("pe", lambda e: e.matmul(Z[:], lhsT=Hh[:, c, :], rhs=wb[:, c, :],
                                               start=(c == 0), stop=(c == 15)), reads=[Hh, wb], writes=[Z])
            if stop == 4:
                kb.dma("sp", oo.a[0:128, :], onesf[:], reads=[onesf], final=True); kb.finish(); return nc
            kb.op("act", lambda e: e.activation(out=junk[:, 0:128], in_=Z[:, 0:128], func=AF.Square,
                                                accum_out=Sx[:, 3:4]), reads=[Z], writes=[junk, Sx])
            kb.op("act", lambda e: e.activation(out=junk[:, 128:256], in_=Z[:, 128:256], func=AF.Square,
                                                accum_out=Sx[:, 4:5]), reads=[Z], writes=[junk, Sx])
            kb.op("act", lambda e: e.activation(out=Sx[:, 3:5], in_=Sx[:, 3:5], func=AF.Sqrt,
                                                scale=1.0 / HD, bias=EPS), reads=[Sx], writes=[Sx])
            kb.op("dve", lambda e: e.reciprocal(out=Sx[:, 5:7], in_=Sx[:, 3:5]), reads=[Sx], writes=[Sx])
            kb.op("dve", lambda e: e.scalar_tensor_tensor(out=Qn[:, 0, :], in0=Z[:, 0:128], scalar=Sx[:, 5:6], in1=gqb[:],
                                                          op0=ALU.mult, op1=ALU.mult),
                  reads=[Z, Sx, gqb], writes=[Qn])
            kb.op("dve", lambda e: e.scalar_tensor_tensor(out=O3[:, 0, :], in0=Z[:, 128:256], scalar=Sx[:, 6:7],
                                                          in1=gkb[:], op0=ALU.mult, op1=ALU.mult),
                  reads=[Z, Sx, gkb], writes=[O3])
            kb.op("act", lambda e: e.copy(out=O3[:, 1:3, :], in_=Z[:, 256:512].rearrange("p (a b) -> p a b", a=2)),
                  reads=[Z], writes=[O3])
            kb.op("pool", lambda e: e.tensor_copy(out=Vx[:, i, 0:HD], in_=O3[:, 1, :]), reads=[O3], writes=[Vx])
            if i < 64:
                kb.dma("sp", kvu.a[i * 128:(i + 1) * 128, :, :], O3[:], reads=[O3], final=True)
            else:
                kb.dma("sp", kvu.a[i * 128:(i + 1) * 128, :, :], O3[:], reads=[O3], writes=[kvu_s], final=True)
            if stop == 5:
                kb.finish(); return nc
            kb.op("pool", lambda e: e.tensor_copy(out=Qn[:, 1, :], in_=O3[:, 0, :]), reads=[O3], writes=[Qn])
            if sub == 1:
                kb.finish(); return nc
            kb.op("pe", lambda e: e.transpose(out=tq[:, 0, :], in_=Qn[:, 0, :], identity=identb[:]),
                  reads=[Qn, identb], writes=[tq])
            kb.op("pe", lambda e: e.transpose(out=tq[:, 1, :], in_=Qn[:, 1, :], identity=identb[:]),
                  reads=[Qn, identb], writes=[tq])
            if sub == 2:
                kb.finish(); return nc
            kb.op("act", lambda e: e.copy(out=QT[:, i * 128:(i + 1) * 128], in_=tq[:, 0, :]), reads=[tq], writes=[QT])
            if sub == 3:
                kb.finish(); return nc
            kb.op("act", lambda e: e.copy(out=KT[:, i * 128:(i + 1) * 128], in_=tq[:, 1, :]),
                  reads=[tq], writes=[KT])
            if stop == 6:
                kb.finish(); return nc
            if i < 64:
                kb.op("dve", lambda e: e.reduce_sum(out=ksum[:, i:i + 1], in_=KT[:, i * 128:(i + 1) * 128], axis=AX.X),
                      reads=[KT], writes=[ksum])
            if stop == 7:
                kb.finish(); return nc
        kv = ksum[:, :].rearrange("p (n t) -> p n t", t=2)
        kb.op("dve", lambda e: e.tensor_tensor(out=kmT[:], in0=kv[:, :, 0], in1=kv[:, :, 1], op=ALU.add),
              reads=[ksum], writes=[kmT])

        gps = kb.ps("gps", 4, 256, 32)
        sps = [kb.ps("sps%d" % i, i, 0, 256, split=2) for i in range(2)]
        ops = [kb.ps("ops%d" % i, 2 + i, 0, HD + 1) for i in range(2)]
        gpad = kb.sb("gpad", [128, 32], F32)
        m8 = kb.sb("m8", [128, 8], F32)
        sel = [kb.sb("sel%d" % i, [128, 32], F32) for i in range(2)]
        pT = [kb.sb("pT%d" % i, [128, 2, 128], BF16) for i in range(3)]
        acc = [kb.sb("acc%d" % i, [128, HD + 1], F32) for i in range(2)]
        ob = [kb.sb("ob%d" % i, [128, HD], F32) for i in range(2)]
        rc = [kb.sb("rc%d" % i, [128, 1], F32) for i in range(2)]
        kb.op("pool", lambda e: e.memset(gpad[:], NEG), writes=[gpad])
        it = 0
        for c in range(nchunks):
            own = c // 2
            qs = slice(c * 128, (c + 1) * 128)
            A, SEL, OB, RC = acc[c % 2], sel[c % 2], ob[c % 2], rc[c % 2]
            if own > 3:
                kb.op("pe", lambda e: e.matmul(gps[:], lhsT=QT[:, qs], rhs=kmT[:], start=True, stop=True),
                      reads=[QT, kmT], writes=[gps])
                kb.op("dve", lambda e: e.tensor_copy(out=gpad[:, 0:own], in_=gps[:, 0:own]), reads=[gps], writes=[gpad])
                w8 = max(own, 8)
                kb.op("dve", lambda e: e.max(out=m8[:], in_=gpad[:, 0:w8]), reads=[gpad], writes=[m8])
                kb.op("dve", lambda e: e.tensor_scalar(out=SEL[:, 0:own], in0=gpad[:, 0:own], scalar1=m8[:, 2:3],
                                                       scalar2=None, op0=ALU.is_ge), reads=[gpad, m8], writes=[SEL])
            for n in [own] + list(range(own)):
                if n == own:
                    kts = list(range(2 * own, c + 1))
                else:
                    kts = [2 * n, 2 * n + 1]
                SP, P, OP = sps[it % 2], pT[it % 3], ops[it % 2]
                it += 1
                for j, kt in enumerate(kts):
                    kb.op("pe", lambda e: e.matmul(SP[:, j, :], lhsT=KT[:, kt * 128:(kt + 1) * 128], rhs=QT[:, qs],
                                                   start=True, stop=True), reads=[KT, QT], writes=[SP])
                nj = len(kts)
                kb.op("act", lambda e: e.activation(out=P[:, 0:nj, :], in_=SP[:, 0:nj, :], func=AF.Exp, scale=SCALE),
                      reads=[SP], writes=[P])
                if n == own:
                    kb.op("pool", lambda e: e.tensor_tensor(out=P[:, nj - 1, :], in0=P[:, nj - 1, :], in1=trib[:],
                                                            op=ALU.mult), reads=[P, trib], writes=[P])
                for j, kt in enumerate(kts):
                    kb.op("pe", lambda e: e.matmul(OP[:], lhsT=P[:, j, :], rhs=Vx[:, kt, :],
                                                   start=(j == 0), stop=(j == nj - 1)), reads=[P, Vx], writes=[OP])
                if n == own:
                    kb.op("act", lambda e: e.copy(out=A[:], in_=OP[:]), reads=[OP], writes=[A])
                elif own > 3:
                    kb.op("dve", lambda e: e.scalar_tensor_tensor(out=A[:], in0=OP[:], scalar=SEL[:, n:n + 1], in1=A[:],
                                                                  op0=ALU.mult, op1=ALU.add),
                          reads=[OP, SEL, A], writes=[A])
                else:
                    kb.op("dve", lambda e: e.tensor_tensor(out=A[:], in0=OP[:], in1=A[:], op=ALU.add),
                          reads=[OP, A], writes=[A])
            kb.op("dve", lambda e: e.reciprocal(out=RC[:], in_=A[:, HD:HD + 1]), reads=[A], writes=[RC])
            kb.op("dve", lambda e: e.tensor_scalar(out=OB[:], in0=A[:, 0:HD], scalar1=RC[:, 0:1], scalar2=None,
                                                   op0=ALU.mult), reads=[A, RC], writes=[OB])
            kb.dma("sp", oo.a[c * 128:(c + 1) * 128, :], OB[:], reads=[OB], final=True)

        KcT = [kb.sb("KcT%d" % i, [128, NPAGES * 128], BF16) for i in range(2)]
        KcF = [kb.sb("KcF%d" % i, [128, NPAGES * 128], F32) for i in range(2)]
        VcF = [kb.sb("VcF%d" % i, [128, NPAGES, HD], F32) for i in range(2)]
        Vc = [kb.sb("Vc%d" % i, [128, NPAGES, HD + 1], BF16) for i in range(2)]
        vsb = [kb.sb("vsb%d" % i, [TS, HD + 1], BF16) for i in range(2)]
        tri4 = kb.sb("tri4", [TS, TS], F32)
        kmb = [kb.sb("kmb%d" % i, [128, 8], BF16) for i in range(2)]
        kmf = [kb.sb("kmf%d" % i, [128, 8], F32) for i in range(2)]
        g8 = [kb.sb("g8%d" % i, [TS, 8], F32) for i in range(2)]
        m8s = [kb.sb("m8s%d" % i, [TS, 8], F32) for i in range(2)]
        sels = [kb.sb("sels%d" % i, [TS, 8], F32) for i in range(2)]
        pTs = [kb.sb("pTs%d" % i, [128, NPAGES, TS], BF16) for i in range(2)]
        pn = [kb.sb("pn%d" % i, [TS, TS], BF16) for i in range(2)]
        pnf = [kb.sb("pnf%d" % i, [TS, TS], F32) for i in range(2)]
        accs = [kb.sb("accs%d" % i, [TS, HD + 1], F32) for i in range(2)]
        obs = [kb.sb("obs%d" % i, [TS, HD], F32) for i in range(2)]
        rcs = [kb.sb("rcs%d" % i, [TS, 1], F32) for i in range(2)]
        ssp = [kb.ps("ssp%d" % i, 5 + i, 0, 64, split=NPAGES) for i in range(2)]
        gsp = kb.ps("gsp", 7, 128, 8, parts=TS)
        snp = kb.ps("snp", 7, 136, TS, parts=TS)
        onp = kb.ps("onp", 7, 160, HD + 1, parts=TS)
        osp = [kb.ps("osp%d" % i, i, 0, HD + 1, parts=TS) for i in range(2)]
        for i in range(2):
            kb.op("pool", lambda e: e.memset(Vc[i][:, :, HD:HD + 1], 1.0), writes=[Vc[i]])
            kb.op("pool", lambda e: e.memset(vsb[i][:, HD:HD + 1], 1.0), writes=[vsb[i]])
        kb.op("pool", lambda e: e.tensor_copy(out=tri4[:], in_=trib[0:TS, 0:TS]), reads=[trib], writes=[tri4])
        it2 = 0
        for b in range(nseq):
            g, bl = b // 8, b % 8
            i2 = b % 2
            KC, VC, VS, KM, G8, M8, SL, PS_, PN, PNF, AS, OBS, RCS, SSP = (
                KcT[i2], Vc[i2], vsb[i2], kmb[i2], g8[i2], m8s[i2], sels[i2], pTs[i2], pn[i2], pnf[i2],
                accs[i2], obs[i2], rcs[i2], ssp[i2])
            cs = slice(S + b * TS, S + (b + 1) * TS)
            KF, VF = KcF[i2], VcF[i2]
            kb.dma("sp", KF[:, :].rearrange("d (j t) -> d j t", j=NPAGES),
                   scrK[g].a[bl * 16:(bl + 1) * 16, :].rearrange("j (d t) -> d j t", d=HD),
                   reads=[scrK[g]], writes=[KF])
            kb.dma("sp", VF[:, :, :],
                   scrV[g].a[bl * 16:(bl + 1) * 16, :].rearrange("j (t d) -> t j d", t=128),
                   reads=[scrV[g]], writes=[VF])
            kb.op("act", lambda e: e.copy(out=KC[:, :], in_=KF[:, :]), reads=[KF], writes=[KC])
            kb.op("pool", lambda e: e.tensor_copy(out=VC[:, :, 0:HD], in_=VF[:, :, :]), reads=[VF], writes=[VC])
            kb.dma("pool", VS[:, 0:HD], kvu.a[S + b * TS:S + (b + 1) * TS, 1, :], reads=[kvu_s], writes=[VS])
            KMF = kmf[i2]
            kb.op("dve", lambda e: e.reduce_sum(out=KMF[:], in_=KC[:, :].rearrange("d (n t) -> d n t", n=8), axis=AX.X),
                  reads=[KC], writes=[KMF])
            kb.op("act", lambda e: e.copy(out=KM[:], in_=KMF[:]), reads=[KMF], writes=[KM])
            kb.op("pe", lambda e: e.matmul(gsp[:], lhsT=QT[:, cs], rhs=KM[:], start=True, stop=True),
                  reads=[QT, KM], writes=[gsp])
            kb.op("dve", lambda e: e.tensor_copy(out=G8[:], in_=gsp[:]), reads=[gsp], writes=[G8])
            kb.op("dve", lambda e: e.max(out=M8[:], in_=G8[:]), reads=[G8], writes=[M8])
            kb.op("dve", lambda e: e.tensor_scalar(out=SL[:], in0=G8[:], scalar1=M8[:, 2:3], scalar2=None,
                                                   op0=ALU.is_ge), reads=[G8, M8], writes=[SL])
            for j in range(NPAGES):
                kb.op("pe", lambda e: e.matmul(SSP[:, j, :], lhsT=KC[:, j * 128:(j + 1) * 128], rhs=QT[:, cs],
                                               start=True, stop=True), reads=[KC, QT], writes=[SSP])
            kb.op("act", lambda e: e.activation(out=PS_[:], in_=SSP[:], func=AF.Exp, scale=SCALE),
                  reads=[SSP], writes=[PS_])
            kb.op("pe", lambda e: e.matmul(snp[:], lhsT=KT[:, cs], rhs=QT[:, cs], start=True, stop=True),
                  reads=[KT, QT], writes=[snp])
            kb.op("act", lambda e: e.activation(out=PNF[:], in_=snp[:], func=AF.Exp, scale=SCALE),
                  reads=[snp], writes=[PNF])
            kb.op("dve", lambda e: e.tensor_tensor(out=PN[:], in0=PNF[:], in1=tri4[:], op=ALU.mult),
                  reads=[PNF, tri4], writes=[PN])
            kb.op("pe", lambda e: e.matmul(onp[:], lhsT=PN[:], rhs=VS[:], start=True, stop=True),
                  reads=[PN, VS], writes=[onp])
            kb.op("act", lambda e: e.copy(out=AS[:], in_=onp[:]), reads=[onp], writes=[AS])
            for n in range(8):
                OP = osp[it2 % 2]
                it2 += 1
                for jj in range(2):
                    j = 2 * n + jj
                    kb.op("pe", lambda e: e.matmul(OP[:], lhsT=PS_[:, j, :], rhs=VC[:, j, :],
                                                   start=(jj == 0), stop=(jj == 1)), reads=[PS_, VC], writes=[OP])
                kb.op("dve", lambda e: e.scalar_tensor_tensor(out=AS[:], in0=OP[:], scalar=SL[:, n:n + 1], in1=AS[:],
                                                              op0=ALU.mult, op1=ALU.add),
                      reads=[OP, SL, AS], writes=[AS])
            kb.op("dve", lambda e: e.reciprocal(out=RCS[:], in_=AS[:, HD:HD + 1]), reads=[AS], writes=[RCS])
            kb.op("dve", lambda e: e.tensor_scalar(out=OBS[:], in0=AS[:, 0:HD], scalar1=RCS[:, 0:1], scalar2=None,
                                                   op0=ALU.mult), reads=[AS, RCS], writes=[OBS])
            kb.dma("sp", oo.a[S + b * TS:S + (b + 1) * TS, :], OBS[:], reads=[OBS], final=True)
        kb.finish()
    return nc


NTB = 9
TB_ROWS = NTB * 128
NE = 16
DF = 768
TGS = [(0, 512), (512, 512), (1024, 128)]


def _barrier(kb, bufs):
    for e, eng in kb.E.items():
        for f in kb.E:
            if f != e and kb.cnt[f] > 0 and kb.seen[e].get(f, 0) < kb.cnt[f]:
                eng.wait_ge(kb.sem[f], kb.cnt[f])
                kb.seen[e][f] = kb.cnt[f]
        for b in bufs:
            if b.dsem is not None and kb.seen[e].get("d_" + b.name, 0) < b.dcnt:
                eng.wait_ge(b.dsem, b.dcnt)
                kb.seen[e]["d_" + b.name] = b.dcnt


def build_ffn(ne=NE, do_moe=True):
    nc = bass.Bass("TRN2", target_bir_lowering=False)
    es = ExitStack()
    with es:
        kb = KB(nc, es)
        kb.banks()
        xc = kb.dr("xc", [TB_ROWS, D], F32, "ExternalInput")
        oaT = kb.dr("oaT", [1024, TB_ROWS], F32, "ExternalInput")
        uTp = kb.dr("uTp", [1024, 15 + 1024], F32, "ExternalInput")
        uTs = kb.dr("uTs", [1024, 16, 19], F32, "ExternalInput")
        invc = kb.dr("invc", [1, 4 * 1024], F32, "ExternalInput")
        wpool = kb.dr("wpool", [4, 256, 256], F32, "ExternalInput")
        pscale = kb.dr("pscale", [128, 8], F32, "ExternalInput")
        wout = kb.dr("wout", [D, D], F32, "ExternalInput")
        gffn = kb.dr("gffn", [1, D], F32, "ExternalInput")
        wr = kb.dr("wr", [D, 20], F32, "ExternalInput")
        br = kb.dr("br", [1, 20], F32, "ExternalInput")
        wgate = kb.dr("wgate", [NE, D, DF], F32, "ExternalInput")
        wup = kb.dr("wup", [NE, D, DF], F32, "ExternalInput")
        wdown = kb.dr("wdown", [NE, DF, D], F32, "ExternalInput")
        y = kb.dr("y", [TB_ROWS, D], F32, "ExternalOutput")
        spool = kb.dr("spool", [176, 1024], F32, "ExternalInput")
        pso = kb.dr("pso", [176, 1024], F32, "ExternalOutput")
        allb = []

        def sb(name, shape, dt, st=None):
            t = TB((st or es).enter_context(nc.sbuf_tensor(name, shape, dt)), name)
            allb.append(t)
            return t

        onesf, identf, identb, trib = _consts(kb)
        allb.extend([onesf, identf, identb, trib])
        Y = sb("Y", [128, NTB, D], F32)
        comb = sb("comb", [128, NTB, NE], F32)
        spt = sb("spt", [128, 2, 1024], F32)
        kb.dma("sp", spt[:, 0, :], spool.a[0:128, :], writes=[spt])
        kb.dma("sp", spt[0:48, 1, :], spool.a[128:176, :], writes=[spt])
        kb.dma("sp", pso.a[0:128, :], spt[:, 0, :], reads=[spt], final=True)
        kb.dma("sp", pso.a[128:176, :], spt[0:48, 1, :], reads=[spt], final=True)
        with ExitStack() as esM:
            mixT = sb("mixT", [128, 16, TB_ROWS], BF16, esM)
            kb.dma("pool", mixT[:, 0:8, :], oaT.a.rearrange("(c p) t -> p c t", p=128), writes=[mixT])
            with ExitStack() as es1:
                dT = sb("dT", [128, 8, TB_ROWS], BF16, es1)
                ivc = sb("ivc", [128, 4, 1024], F32, es1)
                wp = sb("wp", [128, 8, 256], BF16, es1)
                psc = sb("psc", [128, 8], F32, es1)
                Up = [sb("Up%d" % i, [128, 1039], F32, es1) for i in range(2)]
                Ua = [sb("Ua%d" % i, [128, 1039], F32, es1) for i in range(2)]
                Us = [sb("Us%d" % i, [128, 16, 19], F32, es1) for i in range(2)]
                Usa = [sb("Usa%d" % i, [128, 16, 19], F32, es1) for i in range(2)]
                dtmp = sb("dtmp", [128, 1024], F32, es1)
                dtmps = sb("dtmps", [128, 16, 4], F32, es1)
                kb.op("pool", lambda e: e.memset(dT[:], 0.0), writes=[dT])
                kb.dma("sp", ivc[:, :, :].rearrange("p g t -> p (g t)"), invc.a[0, :].partition_broadcast(128), writes=[ivc])
                kb.dma("pool", wp[:], wpool.a.rearrange("g (c p) e -> p (g c) e", p=128), writes=[wp])
                kb.dma("sp", psc[:], pscale.a[:, :], writes=[psc])
                for j in range(8):
                    g = j // 2
                    U, UsJ = Up[j % 2], Us[j % 2]
                    kb.dma("sp", U[:], uTp.a[j * 128:(j + 1) * 128, :], writes=[U])
                    kb.dma("sp", UsJ[:], uTs.a[j * 128:(j + 1) * 128, :, :], writes=[UsJ])
                    cur, curs = U, UsJ
                    lo = 0
                    for k in range(g + 1):
                        sh = 1 << k
                        nxt, nxts = Ua[k % 2], Usa[k % 2]
                        l2 = lo + sh
                        kb.op("dve", lambda e: e.tensor_tensor(out=nxt[:, l2:1039], in0=cur[:, l2:1039],
                                                               in1=cur[:, lo:1039 - sh], op=ALU.add),
                              reads=[cur], writes=[nxt])
                        kb.op("pool", lambda e: e.tensor_tensor(out=nxts[:, :, l2:19], in0=curs[:, :, l2:19],
                                                                in1=curs[:, :, lo:19 - sh], op=ALU.add),
                              reads=[curs], writes=[nxts])
                        cur, curs = nxt, nxts
                        lo = l2
                    w = float(1 << (g + 1))
                    kb.op("dve", lambda e: e.tensor_tensor(out=dtmp[:], in0=cur[:, 15:1039], in1=ivc[:, g, :], op=ALU.mult),
                          reads=[cur, ivc], writes=[dtmp])
                    kb.op("dve", lambda e: e.tensor_tensor(out=dT[:, j, 0:1024], in0=dtmp[:], in1=U[:, 15:1039],
                                                           op=ALU.subtract), reads=[dtmp, U], writes=[dT])
                    kb.op("pool", lambda e: e.tensor_scalar(out=dtmps[:], in0=curs[:, :, 15:19], scalar1=1.0 / w,
                                                            scalar2=None, op0=ALU.mult), reads=[curs], writes=[dtmps])
                    kb.op("pool", lambda e: e.tensor_tensor(
                        out=dT[:, j, 1024:1088].rearrange("p (b t) -> p b t", t=4), in0=dtmps[:],
                        in1=UsJ[:, :, 15:19], op=ALU.subtract), reads=[dtmps, UsJ], writes=[dT])
                pp = [kb.ps("pp%d" % i, i, 0, 512) for i in range(2)]
                it = 0
                for je in range(8):
                    g, el = je // 2, je % 2
                    for (t0, tn) in TGS:
                        P = pp[it % 2]
                        it += 1
                        for cc in range(2):
                            kb.op("pe", lambda e: e.matmul(P[:, 0:tn], lhsT=wp[:, g * 2 + cc, el * 128:(el + 1) * 128],
                                                           rhs=dT[:, g * 2 + cc, t0:t0 + tn], start=(cc == 0), stop=(cc == 1)),
                                  reads=[wp, dT], writes=[P])
                        kb.op("act", lambda e: e.activation(out=mixT[:, 8 + je, t0:t0 + tn], in_=P[:, 0:tn], func=AF.Copy,
                                                            scale=psc[:, je:je + 1]), reads=[P, psc], writes=[mixT])
                _barrier(kb, allb)
            with ExitStack() as es2:
                wo = [sb("wo%d" % i, [128, 16, 512], BF16, es2) for i in range(2)]
                xs = [sb("xs%d" % i, [128, 512], F32, es2) for i in range(3)]
                zp = [kb.ps("zq%d" % i, 2 + i, 0, 512) for i in range(2)]
                it = 0
                for n in range(4):
                    W = wo[n % 2]
                    kb.dma("pool", W[:], wout.a[:, n * 512:(n + 1) * 512].rearrange("(c p) n -> p c n", p=128), writes=[W])
                    for i in range(NTB):
                        X, Z = xs[it % 3], zp[it % 2]
                        it += 1
                        kb.dma("sp", X[:], xc.a[i * 128:(i + 1) * 128, n * 512:(n + 1) * 512], writes=[X])
                        for c in range(16):
                            kb.op("pe", lambda e: e.matmul(Z[:], lhsT=mixT[:, c, i * 128:(i + 1) * 128], rhs=W[:, c, :],
                                                           start=(c == 0), stop=(c == 15)), reads=[mixT, W], writes=[Z])
                        kb.op("dve", lambda e: e.tensor_tensor(out=Y[:, i, n * 512:(n + 1) * 512], in0=Z[:], in1=X[:],
                                                               op=ALU.add), reads=[Z, X], writes=[Y])
                _barrier(kb, allb)
        h2T = sb("h2T", [128, 16, TB_ROWS], BF16)
        with ExitStack() as es3:
            gf = sb("gf", [128, D], F32, es3)
            brb = sb("brb", [128, 20], F32, es3)
            wrf = sb("wrf", [128, 16, 20], F32, es3)
            whi = sb("whi", [128, 16, 20], BF16, es3)
            wlo = sb("wlo", [128, 16, 20], BF16, es3)
            junk = sb("junk2", [128, D], BF16, es3)
            h2f = sb("h2f", [128, D], F32, es3)
            hhi = sb("hhi", [128, D], BF16, es3)
            hlo = sb("hlo", [128, D], BF16, es3)
            loT = sb("loT", [128, 16, 128], BF16, es3)
            st = sb("st3", [128, 8], F32, es3)
            lg = sb("lg", [128, 20], F32, es3)
            r_ = sb("rt", [128, 48], F32, es3)
            tpa = [kb.ps("tpa%d" % i, 4 + i, 0, 512, BF16, split=8) for i in range(2)]
            lgp = kb.ps("lgp", 6, 0, 20)
            kb.dma("sp", gf[:], gffn.a[0, :].partition_broadcast(128), writes=[gf])
            kb.dma("sp", brb[:], br.a[0, :].partition_broadcast(128), writes=[brb])
            kb.dma("sp", wrf[:], wr.a.rearrange("(c p) n -> p c n", p=128), writes=[wrf])
            kb.op("act", lambda e: e.copy(out=whi[:], in_=wrf[:]), reads=[wrf], writes=[whi])
            kb.op("dve", lambda e: e.tensor_tensor(out=wlo[:], in0=wrf[:], in1=whi[:], op=ALU.subtract),
                  reads=[wrf, whi], writes=[wlo])
            for i in range(NTB):
                ts = slice(i * 128, (i + 1) * 128)
                kb.op("act", lambda e: e.activation(out=junk[:], in_=Y[:, i, :], func=AF.Square, accum_out=st[:, 0:1]),
                      reads=[Y], writes=[junk, st])
                kb.op("act", lambda e: e.activation(out=st[:, 1:2], in_=st[:, 0:1], func=AF.Sqrt, scale=1.0 / D, bias=EPS),
                      reads=[st], writes=[st])
                kb.op("dve", lambda e: e.reciprocal(out=st[:, 2:3], in_=st[:, 1:2]), reads=[st], writes=[st])
                kb.op("dve", lambda e: e.scalar_tensor_tensor(out=h2f[:], in0=Y[:, i, :], scalar=st[:, 2:3], in1=gf[:],
                                                              op0=ALU.mult, op1=ALU.mult), reads=[Y, st, gf], writes=[h2f])
                kb.op("act", lambda e: e.copy(out=hhi[:], in_=h2f[:]), reads=[h2f], writes=[hhi])
                kb.op("dve", lambda e: e.tensor_tensor(out=hlo[:], in0=h2f[:], in1=hhi[:], op=ALU.subtract),
                      reads=[h2f, hhi], writes=[hlo])
                for (src, dst, off) in ((hhi, h2T, i * 128), (hlo, loT, 0)):
                    for half in range(2):
                        T = tpa[half]
                        for cc in range(8):
                            c = half * 8 + cc
                            kb.op("pe", lambda e: e.transpose(out=T[:, cc, :], in_=src[:, c * 128:(c + 1) * 128],
                                                              identity=identb[:]), reads=[src, identb], writes=[T])
                        kb.op("act", lambda e: e.copy(out=dst[:, half * 8:(half + 1) * 8, off:off + 128], in_=T[:]),
                              reads=[T], writes=[dst])
                k = 0
                for c in range(16):
                    for (a, b) in ((h2T[:, c, ts], whi), (h2T[:, c, ts], wlo), (loT[:, c, :], whi)):
                        kb.op("pe", lambda e: e.matmul(lgp[:], lhsT=a, rhs=b[:, c, :], start=(k == 0), stop=(k == 47)),
                              reads=[h2T, loT, whi, wlo], writes=[lgp])
                        k += 1
                kb.op("dve", lambda e: e.tensor_tensor(out=lg[:], in0=lgp[:], in1=brb[:], op=ALU.add),
                      reads=[lgp, brb], writes=[lg])
                R = r_
                gl = lg[:, 0:4]
                el3 = lg[:, 4:20].rearrange("p (g e) -> p g e", g=4)
                def dv(fn, reads, writes):
                    kb.op("dve", fn, reads=reads, writes=writes)
                dv(lambda e: e.reduce_max(out=R[:, 0:1], in_=gl, axis=AX.X), [lg], [R])
                dv(lambda e: e.tensor_scalar(out=R[:, 4:8], in0=gl, scalar1=R[:, 0:1], scalar2=None, op0=ALU.subtract), [lg, R], [R])
                kb.op("act", lambda e: e.activation(out=R[:, 8:12], in_=R[:, 4:8], func=AF.Exp, accum_out=R[:, 1:2]),
                      reads=[R], writes=[R])
                dv(lambda e: e.reciprocal(out=R[:, 2:3], in_=R[:, 1:2]), [R], [R])
                dv(lambda e: e.tensor_scalar(out=R[:, 12:16], in0=gl, scalar1=R[:, 0:1], scalar2=None, op0=ALU.is_ge), [lg, R], [R])
                dv(lambda e: e.tensor_tensor(out=R[:, 16:32].rearrange("p (g e) -> p g e", g=4), in0=el3,
                                             in1=R[:, 12:16].unsqueeze(2).to_broadcast([128, 4, 4]), op=ALU.mult), [lg, R], [R])
                dv(lambda e: e.tensor_reduce(out=R[:, 32:36], in_=R[:, 16:32].rearrange("p (g e) -> p e g", g=4),
                                             axis=AX.X, op=ALU.add), [R], [R])
                dv(lambda e: e.reduce_max(out=R[:, 36:37], in_=R[:, 32:36], axis=AX.X), [R], [R])
                dv(lambda e: e.tensor_scalar(out=R[:, 40:44], in0=R[:, 32:36], scalar1=R[:, 36:37], scalar2=NEG,
                                             op0=ALU.is_ge, op1=ALU.mult), [R], [R])
                dv(lambda e: e.tensor_tensor(out=R[:, 40:44], in0=R[:, 40:44], in1=R[:, 32:36], op=ALU.add), [R], [R])
                dv(lambda e: e.reduce_max(out=R[:, 37:38], in_=R[:, 40:44], axis=AX.X), [R], [R])
                dv(lambda e: e.tensor_scalar(out=R[:, 44:48], in0=R[:, 32:36], scalar1=R[:, 37:38], scalar2=None,
                                             op0=ALU.is_ge), [R], [R])
                dv(lambda e: e.tensor_scalar(out=R[:, 40:44], in0=R[:, 32:36], scalar1=R[:, 36:37], scalar2=None,
                                             op0=ALU.subtract), [R], [R])
                kb.op("act", lambda e: e.activation(out=R[:, 40:44], in_=R[:, 40:44], func=AF.Exp), reads=[R], writes=[R])
                dv(lambda e: e.tensor_tensor(out=R[:, 40:44], in0=R[:, 40:44], in1=R[:, 44:48], op=ALU.mult), [R], [R])
                dv(lambda e: e.reduce_sum(out=R[:, 38:39], in_=R[:, 40:44], axis=AX.X), [R], [R])
                dv(lambda e: e.reciprocal(out=R[:, 39:40], in_=R[:, 38:39]), [R], [R])
                dv(lambda e: e.tensor_scalar(out=R[:, 40:44], in0=R[:, 40:44], scalar1=R[:, 39:40], scalar2=R[:, 2:3],
                                             op0=ALU.mult, op1=ALU.mult), [R], [R])
                dv(lambda e: e.tensor_tensor(out=comb[:, i, :].rearrange("p (g e) -> p g e", g=4),
                                             in0=R[:, 12:16].unsqueeze(2).to_broadcast([128, 4, 4]),
                                             in1=R[:, 40:44].unsqueeze(1).to_broadcast([128, 4, 4]), op=ALU.mult), [R], [comb])
            _barrier(kb, allb)
        HF = DF // 2
        wg = [sb("wg%d" % i, [128, 16, HF], BF16) for i in range(2)]
        wu = [sb("wu%d" % i, [128, 16, HF], BF16) for i in range(2)]
        wd = [sb("wd%d" % i, [128, 3, D], BF16) for i in range(2)]
        actT = [sb("actT%d" % i, [128, 3, 512], BF16) for i in range(2)]
        sA = [sb("sA%d" % i, [128, 512], BF16) for i in range(2)]
        pa = [kb.ps("pa%d" % i, i, 0, 512) for i in range(2)]
        pb = [kb.ps("pb%d" % i, 2 + i, 0, 512) for i in range(2)]
        pd = [kb.ps("pd%d" % i, 4 + i, 0, 512) for i in range(2)]
        ia = idn = iu = ig = 0
        for ex in range(ne if do_moe else 0):
            for hf in range(2):
                WG, WU, WD = wg[iu % 2], wu[iu % 2], wd[iu % 2]
                iu += 1
                kb.dma("pool", WG[:], wgate.a[ex, :, hf * HF:(hf + 1) * HF].rearrange("(c p) f -> p c f", p=128), writes=[WG])
                kb.dma("pool", WU[:], wup.a[ex, :, hf * HF:(hf + 1) * HF].rearrange("(c p) f -> p c f", p=128), writes=[WU])
                kb.dma("pool", WD[:], wdown.a[ex, hf * HF:(hf + 1) * HF, :].rearrange("(c p) n -> p c n", p=128), writes=[WD])
                for gi, (t0, tn) in enumerate(TGS):
                    AT = actT[ig % 2]
                    ig += 1
                    for fc in range(3):
                        PA, PB_, SA = pa[ia % 2], pb[ia % 2], sA[ia % 2]
                        ia += 1
                        for c in range(16):
                            kb.op("pe", lambda e: e.matmul(PA[:, 0:tn], lhsT=WG[:, c, fc * 128:(fc + 1) * 128],
                                                           rhs=h2T[:, c, t0:t0 + tn], start=(c == 0), stop=(c == 15)),
                                  reads=[WG, h2T], writes=[PA])
                        for c in range(16):
                            kb.op("pe", lambda e: e.matmul(PB_[:, 0:tn], lhsT=WU[:, c, fc * 128:(fc + 1) * 128],
                                                           rhs=h2T[:, c, t0:t0 + tn], start=(c == 0), stop=(c == 15)),
                                  reads=[WU, h2T], writes=[PB_])
                        kb.op("act", lambda e: e.activation(out=SA[:, 0:tn], in_=PA[:, 0:tn], func=AF.Silu),
                              reads=[PA], writes=[SA])
                        kb.op("dve", lambda e: e.tensor_tensor(out=AT[:, fc, 0:tn], in0=PB_[:, 0:tn], in1=SA[:, 0:tn],
                                                               op=ALU.mult), reads=[PB_, SA], writes=[AT])
                    for ii in range(tn // 128):
                        i = t0 // 128 + ii
                        for n in range(4):
                            PD = pd[idn % 2]
                            idn += 1
                            for fc in range(3):
                                kb.op("pe", lambda e: e.matmul(PD[:], lhsT=AT[:, fc, ii * 128:(ii + 1) * 128],
                                                               rhs=WD[:, fc, n * 512:(n + 1) * 512], start=(fc == 0), stop=(fc == 2)),
                                      reads=[AT, WD], writes=[PD])
                            kb.op("dve", lambda e: e.scalar_tensor_tensor(
                                out=Y[:, i, n * 512:(n + 1) * 512], in0=PD[:], scalar=comb[:, i, ex:ex + 1],
                                in1=Y[:, i, n * 512:(n + 1) * 512], op0=ALU.mult, op1=ALU.add),
                                reads=[PD, comb, Y], writes=[Y])
        for i in range(NTB):
            kb.dma("sp", y.a[i * 128:(i + 1) * 128, :], Y[:, i, :], reads=[Y], final=True)
        kb.finish()
    return nc


def run_attn(inputs):
    x = np.concatenate([np.asarray(inputs["x_prompt"], np.float32).reshape(S, D),
                        np.asarray(inputs["x_sample"], np.float32).reshape(NS, D)], 0)
    w_in = np.asarray(inputs["w_in"], np.float32)[0]
    ck = np.asarray(inputs["cache_k"], np.float32)[0]
    cv = np.asarray(inputs["cache_v"], np.float32)[0]
    pt = np.ascontiguousarray(np.asarray(inputs["page_table"], np.int32).reshape(-1, 1))
    gmix = np.asarray(inputs["g_mix"], np.float32).reshape(1, D)
    gq = np.asarray(inputs["g_q"], np.float32).reshape(1, HD)
    gk = np.asarray(inputs["g_k"], np.float32).reshape(1, HD)
    in_maps = []
    for h in range(NH):
        cols = np.concatenate([np.arange(j * 1024 + h * HD, j * 1024 + (h + 1) * HD) for j in range(4)])
        in_maps.append({
            "x": x, "wh": np.ascontiguousarray(w_in[:, cols]), "gmix": gmix, "gq": gq, "gk": gk,
            "kc": np.ascontiguousarray(ck[:, :, h, :].transpose(0, 2, 1)).reshape(NPHYS, HD * 128),
            "vc": np.ascontiguousarray(cv[:, :, h, :]).reshape(NPHYS, 128 * HD),
            "pt": pt,
        })
    nc = build_attn()
    res = run_bass_kernel_spmd(nc, in_maps, core_ids=list(range(8)))
    kvu = np.stack([np.asarray(r["kvu"]) for r in res.results], 0)
    oo = np.stack([np.asarray(r["oo"]) for r in res.results], 0)
    return kvu, oo


def run_ffn(inputs, kvu, oo):
    POOLW = (2, 4, 8, 16)
    xp = np.asarray(inputs["x_prompt"], np.float32).reshape(S, D)
    xs = np.asarray(inputs["x_sample"], np.float32).reshape(NS, D)
    sp = np.asarray(inputs["state_pool"], np.float32)[0]
    u = np.ascontiguousarray(kvu[:, :, 2, :].transpose(1, 0, 2)).reshape(NTOK, 1024)
    oa = np.ascontiguousarray(oo.transpose(1, 0, 2)).reshape(NTOK, 1024)
    wr = np.ascontiguousarray(np.concatenate([np.asarray(inputs["w_group_router"], np.float32)[0],
                                              np.asarray(inputs["w_expert_router"], np.float32)[0].reshape(D, 16)], 1))
    br = np.concatenate([np.asarray(inputs["b_group_router"], np.float32)[0].reshape(1, 4),
                         np.asarray(inputs["b_expert_router"], np.float32)[0].reshape(1, 16)], 1)
    common = {
        "wpool": np.asarray(inputs["w_pool"], np.float32)[0],
        "pscale": np.ascontiguousarray(np.asarray(inputs["pool_scale"], np.float32)[0].reshape(8, 128).T),
        "wout": np.asarray(inputs["w_out"], np.float32)[0],
        "gffn": np.asarray(inputs["g_ffn"], np.float32).reshape(1, D),
        "wr": wr, "br": np.ascontiguousarray(br),
        "wgate": np.asarray(inputs["w_gate"], np.float32)[0],
        "wup": np.asarray(inputs["w_up"], np.float32)[0],
        "wdown": np.asarray(inputs["w_down"], np.float32)[0],
    }
    in_maps = []
    for c in range(8):
        prow = np.arange(1024 * c, 1024 * (c + 1))
        srow = S + np.arange(64 * c, 64 * (c + 1))
        xc = np.zeros((TB_ROWS, D), np.float32)
        xc[:1024] = xp[prow]
        xc[1024:1088] = xs[64 * c:64 * (c + 1)]
        oaT = np.zeros((1024, TB_ROWS), np.float32)
        oaT[:, :1024] = oa[prow].T
        oaT[:, 1024:1088] = oa[srow].T
        uTp = np.zeros((1024, 1039), np.float32)
        uTp[:, 15:] = u[prow].T
        if c > 0:
            uTp[:, :15] = u[1024 * c - 15:1024 * c].T
        uTs = np.zeros((1024, 16, 19), np.float32)
        uTs[:, :, :15] = sp[16 * c:16 * (c + 1)].transpose(2, 0, 1)
        uTs[:, :, 15:] = u[srow].reshape(16, 4, 1024).transpose(2, 0, 1)
        pos = np.arange(1024 * c, 1024 * (c + 1))
        invc = np.stack([1.0 / np.minimum(float(w), pos + 1.0) for w in POOLW], 0).astype(np.float32).reshape(1, 4096)
        m = dict(common)
        m.update({"xc": xc, "oaT": oaT, "uTp": uTp, "uTs": uTs, "invc": invc,
                  "spool": np.ascontiguousarray(sp[16 * c:16 * (c + 1), 4:15, :]).reshape(176, 1024)})
        in_maps.append(m)
    nc = build_ffn()
    res = run_bass_kernel_spmd(nc, in_maps, core_ids=list(range(8)))
    ys = [np.asarray(r["y"]) for r in res.results]
    pso = [np.asarray(r["pso"]).reshape(16, 11, 1024) for r in res.results]
    return ys, pso, u


def kernel(**inputs):
    kvu, oo = run_attn(inputs)
    ys, pso, u = run_ffn(inputs, kvu, oo)
    f = np.float32
    y_prompt = np.concatenate([y[:1024] for y in ys], 0).reshape(1, S, D).astype(f)
    y_sample = np.concatenate([y[1024:1088] for y in ys], 0).reshape(NSEQ, TS, D).astype(f)
    k_all = np.ascontiguousarray(kvu[:, :, 0, :].transpose(1, 0, 2))
    v_all = np.ascontiguousarray(kvu[:, :, 1, :].transpose(1, 0, 2))
    k_prompt = k_all[:S].reshape(1, 1, S, NH, HD).astype(f)
    v_prompt = v_all[:S].reshape(1, 1, S, NH, HD).astype(f)
    k_sample = k_all[S:].reshape(1, NSEQ, TS, NH, HD).astype(f)
    v_sample = v_all[S:].reshape(1, NSEQ, TS, NH, HD).astype(f)
    pool_prompt = u[S - 15:S].reshape(1, 1, 15, 1024).astype(f)
    pool_sample = np.concatenate([np.concatenate(pso, 0), u[S:].reshape(NSEQ, TS, 1024)], 1).reshape(1, NSEQ, 15, 1024).astype(f)
    return (y_prompt, y_sample, k_prompt, v_prompt, pool_prompt, k_sample, v_sample, pool_sample)
```

```python
import numpy as np
from contextlib import ExitStack
import concourse.bass as bass
import concourse.mybir as mybir
from concourse.bass_utils import run_bass_kernel_spmd

F32 = mybir.dt.float32
BF16 = mybir.dt.bfloat16
I32 = mybir.dt.int32
ALU = mybir.AluOpType
AF = mybir.ActivationFunctionType
AX = mybir.AxisListType

D = 2048
S = 8192
NSEQ = 128
TS = 4
NS = NSEQ * TS
NTOK = S + NS
NT = NTOK // 128
HD = 128
NH = 8
NPAGES = 16
NPHYS = 2560
EPS = 1e-6
SCALE = HD ** -0.5
NEG = -1.0e30


class _St:
    def __init__(self):
        self.w = None
        self.r = {}


class TB:
    def __init__(self, a, name, st=None):
        self.a = a
        self.name = name
        self.s = st if st is not None else _St()
        self.dsem = None
        self.dcnt = 0

    @property
    def w(self):
        return self.s.w

    @w.setter
    def w(self, v):
        self.s.w = v

    @property
    def r(self):
        return self.s.r

    @r.setter
    def r(self, v):
        self.s.r = v

    def __getitem__(self, idx):
        return self.a[idx]


class Tag:
    __slots__ = ("key", "sem", "val", "buf")

    def __init__(self, key, sem, val, buf=None):
        self.key, self.sem, self.val, self.buf = key, sem, val, buf


class KB:
    def __init__(self, nc, es):
        self.nc, self.es = nc, es
        self.E = {"pe": nc.tensor, "act": nc.scalar, "dve": nc.vector, "pool": nc.gpsimd, "sp": nc.sync}
        self.sem = {k: es.enter_context(nc.semaphore("sem_" + k)) for k in self.E}
        self.cnt = {k: 0 for k in self.E}
        self.seen = {k: {} for k in self.E}
        self.outs = []
        self.n = 0

    def sb(self, name, shape, dt):
        return TB(self.es.enter_context(self.nc.sbuf_tensor(name, shape, dt)), name)

    def banks(self):
        self.bk = [self.es.enter_context(self.nc.psum_tensor("bank%d" % i, [128, 512], F32)) for i in range(8)]
        self.bst = [_St() for i in range(8)]

    def ps(self, name, bank, off, n, dt=F32, split=None, parts=128):
        a = self.bk[bank][0:parts, off:off + n]
        if dt == BF16:
            a = a.bitcast(BF16)
        if split is not None:
            a = a.rearrange("p (a b) -> p a b", a=split)
        return TB(a, name, self.bst[bank])

    def dr(self, name, shape, dt, kind):
        return TB(self.nc.dram_tensor(name, shape, dt, kind=kind).ap(), name)

    def _wait(self, e, reads, writes):
        tags = []
        for b in reads:
            if b.w is not None:
                tags.append(b.w)
        for b in writes:
            if b.w is not None:
                tags.append(b.w)
            tags.extend(b.r.values())
        eng = self.E[e]
        for t in tags:
            if e == "pe" and t.key == "pe":
                continue
            val = t.buf.dcnt if t.buf is not None else t.val
            if self.seen[e].get(t.key, 0) < val:
                eng.wait_ge(t.sem, val)
                self.seen[e][t.key] = val

    def op(self, e, fn, reads=(), writes=()):
        self._wait(e, reads, writes)
        ins = fn(self.E[e])
        self.cnt[e] += 1
        ins.then_inc(self.sem[e], 1)
        tag = Tag(e, self.sem[e], self.cnt[e])
        for b in reads:
            b.r[e] = tag
        for b in writes:
            b.w = tag
            b.r = {}
        return ins

    def _dma_done(self, ins, reads, writes, final):
        b = writes[0] if writes else reads[0]
        if b.dsem is None:
            self.n += 1
            b.dsem = self.es.enter_context(self.nc.semaphore("dsem%d" % self.n))
        b.dcnt += 16
        ins.then_inc(b.dsem, 16)
        tag = Tag("d_" + b.name, b.dsem, b.dcnt, b)
        for x in reads:
            x.r[tag.key] = tag
        for x in writes:
            x.w = tag
            x.r = {}
        if final and b not in self.outs:
            self.outs.append(b)

    def dma(self, q, out, in_, reads=(), writes=(), final=False):
        self._wait(q, reads, writes)
        ins = self.E[q].dma_start(out=out, in_=in_)
        self._dma_done(ins, list(reads), list(writes), final)

    def gather(self, out, in_, idx_ap, reads=(), writes=()):
        self._wait("pool", reads, writes)
        ins = self.nc.gpsimd.indirect_dma_start(
            out=out, out_offset=None, in_=in_,
            in_offset=bass.IndirectOffsetOnAxis(ap=idx_ap, axis=0))
        self._dma_done(ins, list(reads), list(writes), False)

    def finish(self):
        for b in self.outs:
            self.nc.sync.wait_ge(b.dsem, b.dcnt)
        for e in self.E:
            if e != "sp" and self.cnt[e] > 0:
                self.nc.sync.wait_ge(self.sem[e], self.cnt[e])


def _consts(kb):
    nc = kb.nc
    onesf = kb.sb("onesf", [128, 128], F32)
    identf = kb.sb("identf", [128, 128], F32)
    identb = kb.sb("identb", [128, 128], BF16)
    trib = kb.sb("trib", [128, 128], BF16)
    kb.op("pool", lambda e: e.memset(onesf[:], 1.0), writes=[onesf])
    kb.op("pool", lambda e: e.affine_select(out=identf[:], in_=onesf[:], pattern=[[1, 128]],
                                            compare_op=ALU.is_equal, fill=0.0, base=0,
                                            channel_multiplier=-1), reads=[onesf], writes=[identf])
    kb.op("pool", lambda e: e.tensor_copy(out=identb[:], in_=identf[:]), reads=[identf], writes=[identb])
    kb.op("pool", lambda e: e.affine_select(out=trib[:], in_=onesf[:], pattern=[[1, 128]],
                                            compare_op=ALU.is_ge, fill=0.0, base=0,
                                            channel_multiplier=-1), reads=[onesf], writes=[trib])
    return onesf, identf, identb, trib


def build_attn(nphys=NPHYS, ngroups=16, tiles=None, nchunks=64, nseq=NSEQ, xrows=NTOK, stop=0, sub=9):
    nc = bass.Bass("TRN2", target_bir_lowering=False)
    es = ExitStack()
    with es:
        kb = KB(nc, es)
        kb.banks()
        x = kb.dr("x", [xrows, D], F32, "ExternalInput")
        wh = kb.dr("wh", [D, 512], F32, "ExternalInput")
        gmix = kb.dr("gmix", [1, D], F32, "ExternalInput")
        gq = kb.dr("gq", [1, HD], F32, "ExternalInput")
        gk = kb.dr("gk", [1, HD], F32, "ExternalInput")
        kc = kb.dr("kc", [nphys, HD * 128], F32, "ExternalInput")
        vc = kb.dr("vc", [nphys, 128 * HD], F32, "ExternalInput")
        pt = kb.dr("pt", [NSEQ * NPAGES, 1], I32, "ExternalInput")
        kvu = kb.dr("kvu", [NTOK, 3, HD], F32, "ExternalOutput")
        oo = kb.dr("oo", [NTOK, HD], F32, "ExternalOutput")
        scrK = [kb.dr("scrK%d" % g, [128, HD * 128], F32, "Internal") for g in range(16)]
        scrV = [kb.dr("scrV%d" % g, [128, HD * 128], F32, "Internal") for g in range(16)]
        kvu_s = TB(kvu.a, "kvu_s")

        onesf, identf, identb, trib = _consts(kb)

        ptt = kb.sb("ptt", [128, 16], I32)
        ptt2 = kb.sb("ptt2", [128, 16, 2], I32)
        stage = kb.sb("stage", [128, HD * 64], F32)
        for g in range(16):
            kb.dma("sp", ptt[:, g:g + 1], pt.a[g * 128:(g + 1) * 128, :], writes=[ptt])
        kb.op("dve", lambda e: e.tensor_scalar(out=ptt2[:, :, 0], in0=ptt[:], scalar1=2.0, scalar2=None,
                                               op0=ALU.mult), reads=[ptt], writes=[ptt2])
        kb.op("dve", lambda e: e.tensor_scalar(out=ptt2[:, :, 1], in0=ptt[:], scalar1=2.0, scalar2=1.0,
                                               op0=ALU.mult, op1=ALU.add), reads=[ptt], writes=[ptt2])
        kc2 = kc.a.rearrange("n (h e) -> (n h) e", h=2)
        vc2 = vc.a.rearrange("n (h e) -> (n h) e", h=2)
        for g in range(ngroups):
            for (src, scr) in ((kc2, scrK[g]), (vc2, scrV[g])):
                for hf in range(2):
                    kb.gather(stage[:], src, ptt2[:, g, hf:hf + 1], reads=[ptt2], writes=[stage])
                    kb.dma("sp", scr.a[:, hf * HD * 64:(hf + 1) * HD * 64], stage[:], reads=[stage], writes=[scr])

        gbc = kb.sb("gbc", [128, D], F32)
        gqb = kb.sb("gqb", [128, HD], F32)
        gkb = kb.sb("gkb", [128, HD], F32)
        wb = kb.sb("wb", [128, 16, 512], BF16)
        kb.dma("sp", gbc[:], gmix.a[0, :].partition_broadcast(128), writes=[gbc])
        kb.dma("sp", gqb[:], gq.a[0, :].partition_broadcast(128), writes=[gqb])
        kb.dma("sp", gkb[:], gk.a[0, :].partition_broadcast(128), writes=[gkb])
        kb.dma("pool", wb[:], wh.a.rearrange("(c p) n -> p c n", p=128), writes=[wb])

        if stop == 1:
            kb.dma("sp", oo.a[0:128, :], onesf[:], reads=[onesf], final=True)
            kb.finish()
            return nc
        QT = kb.sb("QT", [128, NTOK], BF16)
        KT = kb.sb("KT", [128, NTOK], BF16)
        Vx = kb.sb("Vx", [128, NT, HD + 1], BF16)
        ksum = kb.sb("ksum", [128, 64], F32)
        kmT = kb.sb("kmT", [128, 32], BF16)
        kb.op("pool", lambda e: e.memset(Vx[:, :, HD:HD + 1], 1.0), writes=[Vx])
        kb.op("pool", lambda e: e.memset(ksum[:], 0.0), writes=[ksum])

        xt = [kb.sb("xt%d" % i, [128, D], F32) for i in range(2)]
        junk = kb.sb("junk", [128, D], BF16)
        hn = kb.sb("hn", [128, D], BF16)
        hT = [kb.sb("hT%d" % i, [128, 16, 128], BF16) for i in range(2)]
        st = [kb.sb("st%d" % i, [128, 8], F32) for i in range(2)]
        o3 = [kb.sb("o3%d" % i, [128, 3, HD], F32) for i in range(2)]
        qn = [kb.sb("qn%d" % i, [128, 2, HD], BF16) for i in range(2)]
        zp = [kb.ps("zp%d" % i, i, 0, 512) for i in range(2)]
        tp = [kb.ps("tp%d" % i, 2 + i, 0, 512, BF16, split=8) for i in range(2)]
        tq = kb.ps("tq", 4, 0, 128, BF16, split=2)

        tl = list(range(NT) if tiles is None else tiles)
        if tl:
            kb.dma("sp", xt[tl[0] % 2][:], x.a[tl[0] * 128:(tl[0] + 1) * 128, :], writes=[xt[tl[0] % 2]])
        for ti, i in enumerate(tl):
            X, Hh, Sx, O3, Qn, Z = xt[i % 2], hT[i % 2], st[i % 2], o3[i % 2], qn[i % 2], zp[i % 2]
            if ti + 1 < len(tl):
                i1 = tl[ti + 1]
                kb.dma("sp", xt[i1 % 2][:], x.a[i1 * 128:(i1 + 1) * 128, :], writes=[xt[i1 % 2]])
            kb.op("act", lambda e: e.activation(out=junk[:], in_=X[:], func=AF.Square,
                                                accum_out=Sx[:, 0:1]), reads=[X], writes=[junk, Sx])
            kb.op("act", lambda e: e.activation(out=Sx[:, 1:2], in_=Sx[:, 0:1], func=AF.Sqrt,
                                                scale=1.0 / D, bias=EPS), reads=[Sx], writes=[Sx])
            kb.op("dve", lambda e: e.reciprocal(out=Sx[:, 2:3], in_=Sx[:, 1:2]), reads=[Sx], writes=[Sx])
            kb.op("dve", lambda e: e.scalar_tensor_tensor(out=hn[:], in0=X[:], scalar=Sx[:, 2:3], in1=gbc[:],
                                                          op0=ALU.mult, op1=ALU.mult),
                  reads=[X, Sx, gbc], writes=[hn])
            if stop == 2:
                kb.dma("sp", oo.a[0:128, :], onesf[:], reads=[onesf], final=True); kb.finish(); return nc
            for half in range(2):
                T = tp[half]
                for cc in range(8):
                    c = half * 8 + cc
                    kb.op("pe", lambda e: e.transpose(out=T[:, cc, :], in_=hn[:, c * 128:(c + 1) * 128],
                                                      identity=identb[:]), reads=[hn, identb], writes=[T])
                if half == 0:
                    kb.op("act", lambda e: e.copy(out=Hh[:, 0:8, :], in_=T[:]), reads=[T], writes=[Hh])
                else:
                    kb.op("dve", lambda e: e.tensor_copy(out=Hh[:, 8:16, :], in_=T[:]), reads=[T], writes=[Hh])
            if stop == 3:
                kb.dma("sp", oo.a[0:128, :], onesf[:], reads=[onesf], final=True); kb.finish(); return nc
            for c in range(16):
                kb.op("pe", lambda e: e.matmul(Z[:], lhsT=Hh[:, c, :], rhs=wb[:, c, :],
                                               start=(c == 0), stop=(c == 15)), reads=[Hh, wb], writes=[Z])
            if stop == 4:
                kb.dma("sp", oo.a[0:128, :], onesf[:], reads=[onesf], final=True); kb.finish(); return nc
            kb.op("act", lambda e: e.activation(out=junk[:, 0:128], in_=Z[:, 0:128], func=AF.Square,
                                                accum_out=Sx[:, 3:4]), reads=[Z], writes=[junk, Sx])
            kb.op("act", lambda e: e.activation(out=junk[:, 128:256], in_=Z[:, 128:256], func=AF.Square,
                                                accum_out=Sx[:, 4:5]), reads=[Z], writes=[junk, Sx])
            kb.op("act", lambda e: e.activation(out=Sx[:, 3:5], in_=Sx[:, 3:5], func=AF.Sqrt,
                                                scale=1.0 / HD, bias=EPS), reads=[Sx], writes=[Sx])
            kb.op("dve", lambda e: e.reciprocal(out=Sx[:, 5:7], in_=Sx[:, 3:5]), reads=[Sx], writes=[Sx])
            kb.op("dve", lambda e: e.scalar_tensor_tensor(out=Qn[:, 0, :], in0=Z[:, 0:128], scalar=Sx[:, 5:6], in1=gqb[:],
                                                          op0=ALU.mult, op1=ALU.mult),
                  reads=[Z, Sx, gqb], writes=[Qn])
            kb.op("dve", lambda e: e.scalar_tensor_tensor(out=O3[:, 0, :], in0=Z[:, 128:256], scalar=Sx[:, 6:7],
                                                          in1=gkb[:], op0=ALU.mult, op1=ALU.mult),
                  reads=[Z, Sx, gkb], writes=[O3])
            kb.op("act", lambda e: e.copy(out=O3[:, 1:3, :], in_=Z[:, 256:512].rearrange("p (a b) -> p a b", a=2)),
                  reads=[Z], writes=[O3])
            kb.op("pool", lambda e: e.tensor_copy(out=Vx[:, i, 0:HD], in_=O3[:, 1, :]), reads=[O3], writes=[Vx])
            if i < 64:
                kb.dma("sp", kvu.a[i * 128:(i + 1) * 128, :, :], O3[:], reads=[O3], final=True)
            else:
                kb.dma("sp", kvu.a[i * 128:(i + 1) * 128, :, :], O3[:], reads=[O3], writes=[kvu_s], final=True)
            if stop == 5:
                kb.finish(); return nc
            kb.op("pool", lambda e: e.tensor_copy(out=Qn[:, 1, :], in_=O3[:, 0, :]), reads=[O3], writes=[Qn])
            if sub == 1:
                kb.finish(); return nc
            kb.op("pe", lambda e: e.transpose(out=tq[:, 0, :], in_=Qn[:, 0, :], identity=identb[:]),
                  reads=[Qn, identb], writes=[tq])
            kb.op("pe", lambda e: e.transpose(out=tq[:, 1, :], in_=Qn[:, 1, :], identity=identb[:]),
                  reads=[Qn, identb], writes=[tq])
            if sub == 2:
                kb.finish(); return nc
            kb.op("act", lambda e: e.copy(out=QT[:, i * 128:(i + 1) * 128], in_=tq[:, 0, :]), reads=[tq], writes=[QT])
            if sub == 3:
                kb.finish(); return nc
            kb.op("act", lambda e: e.copy(out=KT[:, i * 128:(i + 1) * 128], in_=tq[:, 1, :]),
                  reads=[tq], writes=[KT])
            if stop == 6:
                kb.finish(); return nc
            if i < 64:
                kb.op("dve", lambda e: e.reduce_sum(out=ksum[:, i:i + 1], in_=KT[:, i * 128:(i + 1) * 128], axis=AX.X),
                      reads=[KT], writes=[ksum])
            if stop == 7:
                kb.finish(); return nc
        kv = ksum[:, :].rearrange("p (n t) -> p n t", t=2)
        kb.op("dve", lambda e: e.tensor_tensor(out=kmT[:], in0=kv[:, :, 0], in1=kv[:, :, 1], op=ALU.add),
              reads=[ksum], writes=[kmT])

        gps = kb.ps("gps", 4, 256, 32)
        sps = [kb.ps("sps%d" % i, i, 0, 256, split=2) for i in range(2)]
        ops = [kb.ps("ops%d" % i, 2 + i, 0, HD + 1) for i in range(2)]
        gpad = kb.sb("gpad", [128, 32], F32)
        m8 = kb.sb("m8", [128, 8], F32)
        sel = [kb.sb("sel%d" % i, [128, 32], F32) for i in range(2)]
        pT = [kb.sb("pT%d" % i, [128, 2, 128], BF16) for i in range(3)]
        acc = [kb.sb("acc%d" % i, [128, HD + 1], F32) for i in range(2)]
        ob = [kb.sb("ob%d" % i, [128, HD], F32) for i in range(2)]
        rc = [kb.sb("rc%d" % i, [128, 1], F32) for i in range(2)]
        kb.op("pool", lambda e: e.memset(gpad[:], NEG), writes=[gpad])
        it = 0
        for c in range(nchunks):
            own = c // 2
            qs = slice(c * 128, (c + 1) * 128)
            A, SEL, OB, RC = acc[c % 2], sel[c % 2], ob[c % 2], rc[c % 2]
            if own > 3:
                kb.op("pe", lambda e: e.matmul(gps[:], lhsT=QT[:, qs], rhs=kmT[:], start=True, stop=True),
                      reads=[QT, kmT], writes=[gps])
                kb.op("dve", lambda e: e.tensor_copy(out=gpad[:, 0:own], in_=gps[:, 0:own]), reads=[gps], writes=[gpad])
                w8 = max(own, 8)
                kb.op("dve", lambda e: e.max(out=m8[:], in_=gpad[:, 0:w8]), reads=[gpad], writes=[m8])
                kb.op("dve", lambda e: e.tensor_scalar(out=SEL[:, 0:own], in0=gpad[:, 0:own], scalar1=m8[:, 2:3],
                                                       scalar2=None, op0=ALU.is_ge), reads=[gpad, m8], writes=[SEL])
            for n in [own] + list(range(own)):
                if n == own:
                    kts = list(range(2 * own, c + 1))
                else:
                    kts = [2 * n, 2 * n + 1]
                SP, P, OP = sps[it % 2], pT[it % 3], ops[it % 2]
                it += 1
                for j, kt in enumerate(kts):
                    kb.op("pe", lambda e: e.matmul(SP[:, j, :], lhsT=KT[:, kt * 128:(kt + 1) * 128], rhs=QT[:, qs],
                                                   start=True, stop=True), reads=[KT, QT], writes=[SP])
                nj = len(kts)
                kb.op("act", lambda e: e.activation(out=P[:, 0:nj, :], in_=SP[:, 0:nj, :], func=AF.Exp, scale=SCALE),
                      reads=[SP], writes=[P])
                if n == own:
                    kb.op("pool", lambda e: e.tensor_tensor(out=P[:, nj - 1, :], in0=P[:, nj - 1, :], in1=trib[:],
                                                            op=ALU.mult), reads=[P, trib], writes=[P])
                for j, kt in enumerate(kts):
                    kb.op("pe", lambda e: e.matmul(OP[:], lhsT=P[:, j, :], rhs=Vx[:, kt, :],
                                                   start=(j == 0), stop=(j == nj - 1)), reads=[P, Vx], writes=[OP])
                if n == own:
                    kb.op("act", lambda e: e.copy(out=A[:], in_=OP[:]), reads=[OP], writes=[A])
                elif own > 3:
                    kb.op("dve", lambda e: e.scalar_tensor_tensor(out=A[:], in0=OP[:], scalar=SEL[:, n:n + 1], in1=A[:],
                                                                  op0=ALU.mult, op1=ALU.add),
                          reads=[OP, SEL, A], writes=[A])
                else:
                    kb.op("dve", lambda e: e.tensor_tensor(out=A[:], in0=OP[:], in1=A[:], op=ALU.add),
                          reads=[OP, A], writes=[A])
            kb.op("dve", lambda e: e.reciprocal(out=RC[:], in_=A[:, HD:HD + 1]), reads=[A], writes=[RC])
            kb.op("dve", lambda e: e.tensor_scalar(out=OB[:], in0=A[:, 0:HD], scalar1=RC[:, 0:1], scalar2=None,
                                                   op0=ALU.mult), reads=[A, RC], writes=[OB])
            kb.dma("sp", oo.a[c * 128:(c + 1) * 128, :], OB[:], reads=[OB], final=True)

        KcT = [kb.sb("KcT%d" % i, [128, NPAGES * 128], BF16) for i in range(2)]
        KcF = [kb.sb("KcF%d" % i, [128, NPAGES * 128], F32) for i in range(2)]
        VcF = [kb.sb("VcF%d" % i, [128, NPAGES, HD], F32) for i in range(2)]
        Vc = [kb.sb("Vc%d" % i, [128, NPAGES, HD + 1], BF16) for i in range(2)]
        vsb = [kb.sb("vsb%d" % i, [TS, HD + 1], BF16) for i in range(2)]
        tri4 = kb.sb("tri4", [TS, TS], F32)
        kmb = [kb.sb("kmb%d" % i, [128, 8], BF16) for i in range(2)]
        kmf = [kb.sb("kmf%d" % i, [128, 8], F32) for i in range(2)]
        g8 = [kb.sb("g8%d" % i, [TS, 8], F32) for i in range(2)]
        m8s = [kb.sb("m8s%d" % i, [TS, 8], F32) for i in range(2)]
        sels = [kb.sb("sels%d" % i, [TS, 8], F32) for i in range(2)]
        pTs = [kb.sb("pTs%d" % i, [128, NPAGES, TS], BF16) for i in range(2)]
        pn = [kb.sb("pn%d" % i, [TS, TS], BF16) for i in range(2)]
        pnf = [kb.sb("pnf%d" % i, [TS, TS], F32) for i in range(2)]
        accs = [kb.sb("accs%d" % i, [TS, HD + 1], F32) for i in range(2)]
        obs = [kb.sb("obs%d" % i, [TS, HD], F32) for i in range(2)]
        rcs = [kb.sb("rcs%d" % i, [TS, 1], F32) for i in range(2)]
        ssp = [kb.ps("ssp%d" % i, 5 + i, 0, 64, split=NPAGES) for i in range(2)]
        gsp = kb.ps("gsp", 7, 128, 8, parts=TS)
        snp = kb.ps("snp", 7, 136, TS, parts=TS)
        onp = kb.ps("onp", 7, 160, HD + 1, parts=TS)
        osp = [kb.ps("osp%d" % i, i, 0, HD + 1, parts=TS) for i in range(2)]
        for i in range(2):
            kb.op("pool", lambda e: e.memset(Vc[i][:, :, HD:HD + 1], 1.0), writes=[Vc[i]])
            kb.op("pool", lambda e: e.memset(vsb[i][:, HD:HD + 1], 1.0), writes=[vsb[i]])
        kb.op("pool", lambda e: e.tensor_copy(out=tri4[:], in_=trib[0:TS, 0:TS]), reads=[trib], writes=[tri4])
        def ldseq(bb):
            gg, bbl, j2 = bb // 8, bb % 8, bb % 2
            kb.dma("sp", KcF[j2][:, :].rearrange("d (j t) -> d j t", j=NPAGES),
                   scrK[gg].a[bbl * 16:(bbl + 1) * 16, :].rearrange("j (d t) -> d j t", d=HD),
                   reads=[scrK[gg]], writes=[KcF[j2]])
            kb.dma("sp", VcF[j2][:, :, :],
                   scrV[gg].a[bbl * 16:(bbl + 1) * 16, :].rearrange("j (t d) -> t j d", t=128),
                   reads=[scrV[gg]], writes=[VcF[j2]])
            kb.dma("pool", vsb[j2][:, 0:HD], kvu.a[S + bb * TS:S + (bb + 1) * TS, 1, :], reads=[kvu_s], writes=[vsb[j2]])

        it2 = 0
        for b in range(nseq):
            g, bl = b // 8, b % 8
            i2 = b % 2
            KC, VC, VS, KM, G8, M8, SL, PS_, PN, PNF, AS, OBS, RCS, SSP = (
                KcT[i2], Vc[i2], vsb[i2], kmb[i2], g8[i2], m8s[i2], sels[i2], pTs[i2], pn[i2], pnf[i2],
                accs[i2], obs[i2], rcs[i2], ssp[i2])
            cs = slice(S + b * TS, S + (b + 1) * TS)
            KF, VF = KcF[i2], VcF[i2]
            if b == 0:
                ldseq(0)
            if b + 1 < nseq:
                ldseq(b + 1)
            kb.op("act", lambda e: e.copy(out=KC[:, :], in_=KF[:, :]), reads=[KF], writes=[KC])
            kb.op("pool", lambda e: e.tensor_copy(out=VC[:, :, 0:HD], in_=VF[:, :, :]), reads=[VF], writes=[VC])
            KMF = kmf[i2]
            kb.op("dve", lambda e: e.reduce_sum(out=KMF[:], in_=KC[:, :].rearrange("d (n t) -> d n t", n=8), axis=AX.X),
                  reads=[KC], writes=[KMF])
            kb.op("act", lambda e: e.copy(out=KM[:], in_=KMF[:]), reads=[KMF], writes=[KM])
            kb.op("pe", lambda e: e.matmul(gsp[:], lhsT=QT[:, cs], rhs=KM[:], start=True, stop=True),
                  reads=[QT, KM], writes=[gsp])
            kb.op("dve", lambda e: e.tensor_copy(out=G8[:], in_=gsp[:]), reads=[gsp], writes=[G8])
            kb.op("dve", lambda e: e.max(out=M8[:], in_=G8[:]), reads=[G8], writes=[M8])
            kb.op("dve", lambda e: e.tensor_scalar(out=SL[:], in0=G8[:], scalar1=M8[:, 2:3], scalar2=None,
                                                   op0=ALU.is_ge), reads=[G8, M8], writes=[SL])
            for j in range(NPAGES):
                kb.op("pe", lambda e: e.matmul(SSP[:, j, :], lhsT=KC[:, j * 128:(j + 1) * 128], rhs=QT[:, cs],
                                               start=True, stop=True), reads=[KC, QT], writes=[SSP])
            kb.op("act", lambda e: e.activation(out=PS_[:], in_=SSP[:], func=AF.Exp, scale=SCALE),
                  reads=[SSP], writes=[PS_])
            kb.op("pe", lambda e: e.matmul(snp[:], lhsT=KT[:, cs], rhs=QT[:, cs], start=True, stop=True),
                  reads=[KT, QT], writes=[snp])
            kb.op("act", lambda e: e.activation(out=PNF[:], in_=snp[:], func=AF.Exp, scale=SCALE),
                  reads=[snp], writes=[PNF])
            kb.op("dve", lambda e: e.tensor_tensor(out=PN[:], in0=PNF[:], in1=tri4[:], op=ALU.mult),
                  reads=[PNF, tri4], writes=[PN])
            kb.op("pe", lambda e: e.matmul(onp[:], lhsT=PN[:], rhs=VS[:], start=True, stop=True),
                  reads=[PN, VS], writes=[onp])
            kb.op("act", lambda e: e.copy(out=AS[:], in_=onp[:]), reads=[onp], writes=[AS])
            for n in range(8):
                OP = osp[it2 % 2]
                it2 += 1
                for jj in range(2):
                    j = 2 * n + jj
                    kb.op("pe", lambda e: e.matmul(OP[:], lhsT=PS_[:, j, :], rhs=VC[:, j, :],
                                                   start=(jj == 0), stop=(jj == 1)), reads=[PS_, VC], writes=[OP])
                kb.op("dve", lambda e: e.scalar_tensor_tensor(out=AS[:], in0=OP[:], scalar=SL[:, n:n + 1], in1=AS[:],
                                                              op0=ALU.mult, op1=ALU.add),
                      reads=[OP, SL, AS], writes=[AS])
            kb.op("dve", lambda e: e.reciprocal(out=RCS[:], in_=AS[:, HD:HD + 1]), reads=[AS], writes=[RCS])
            kb.op("dve", lambda e: e.tensor_scalar(out=OBS[:], in0=AS[:, 0:HD], scalar1=RCS[:, 0:1], scalar2=None,
                                                   op0=ALU.mult), reads=[AS, RCS], writes=[OBS])
            kb.dma("sp", oo.a[S + b * TS:S + (b + 1) * TS, :], OBS[:], reads=[OBS], final=True)
        kb.finish()
    return nc


NTB = 9
TB_ROWS = NTB * 128
NE = 16
DF = 768
TGS = [(0, 512), (512, 512), (1024, 128)]


def _barrier(kb, bufs):
    for e, eng in kb.E.items():
        for f in kb.E:
            if f != e and kb.cnt[f] > 0 and kb.seen[e].get(f, 0) < kb.cnt[f]:
                eng.wait_ge(kb.sem[f], kb.cnt[f])
                kb.seen[e][f] = kb.cnt[f]
        for b in bufs:
            if b.dsem is not None and kb.seen[e].get("d_" + b.name, 0) < b.dcnt:
                eng.wait_ge(b.dsem, b.dcnt)
                kb.seen[e]["d_" + b.name] = b.dcnt


def build_ffn(ne=NE, do_moe=True):
    nc = bass.Bass("TRN2", target_bir_lowering=False)
    es = ExitStack()
    with es:
        kb = KB(nc, es)
        kb.banks()
        xc = kb.dr("xc", [TB_ROWS, D], F32, "ExternalInput")
        oaT = kb.dr("oaT", [1024, TB_ROWS], F32, "ExternalInput")
        uTp = kb.dr("uTp", [1024, 15 + 1024], F32, "ExternalInput")
        uTs = kb.dr("uTs", [1024, 16, 19], F32, "ExternalInput")
        invc = kb.dr("invc", [1, 4 * 1024], F32, "ExternalInput")
        wpool = kb.dr("wpool", [4, 256, 256], F32, "ExternalInput")
        pscale = kb.dr("pscale", [128, 8], F32, "ExternalInput")
        wout = kb.dr("wout", [D, D], F32, "ExternalInput")
        gffn = kb.dr("gffn", [1, D], F32, "ExternalInput")
        wr = kb.dr("wr", [D, 20], F32, "ExternalInput")
        br = kb.dr("br", [1, 20], F32, "ExternalInput")
        wgate = kb.dr("wgate", [NE, D, DF], F32, "ExternalInput")
        wup = kb.dr("wup", [NE, D, DF], F32, "ExternalInput")
        wdown = kb.dr("wdown", [NE, DF, D], F32, "ExternalInput")
        y = kb.dr("y", [TB_ROWS, D], F32, "ExternalOutput")
        spool = kb.dr("spool", [176, 1024], F32, "ExternalInput")
        pso = kb.dr("pso", [176, 1024], F32, "ExternalOutput")
        allb = []

        def sb(name, shape, dt, st=None):
            t = TB((st or es).enter_context(nc.sbuf_tensor(name, shape, dt)), name)
            allb.append(t)
            return t

        onesf, identf, identb, trib = _consts(kb)
        allb.extend([onesf, identf, identb, trib])
        Y = sb("Y", [128, NTB, D], F32)
        comb = sb("comb", [128, NTB, NE], F32)
        spt = sb("spt", [128, 2, 1024], F32)
        kb.dma("sp", spt[:, 0, :], spool.a[0:128, :], writes=[spt])
        kb.dma("sp", spt[0:48, 1, :], spool.a[128:176, :], writes=[spt])
        kb.dma("sp", pso.a[0:128, :], spt[:, 0, :], reads=[spt], final=True)
        kb.dma("sp", pso.a[128:176, :], spt[0:48, 1, :], reads=[spt], final=True)
        with ExitStack() as esM:
            mixT = sb("mixT", [128, 16, TB_ROWS], BF16, esM)
            kb.dma("pool", mixT[:, 0:8, :], oaT.a.rearrange("(c p) t -> p c t", p=128), writes=[mixT])
            with ExitStack() as es1:
                dT = sb("dT", [128, 8, TB_ROWS], BF16, es1)
                ivc = sb("ivc", [128, 4, 1024], F32, es1)
                wp = sb("wp", [128, 8, 256], BF16, es1)
                psc = sb("psc", [128, 8], F32, es1)
                Up = [sb("Up%d" % i, [128, 1039], F32, es1) for i in range(2)]
                Ua = [sb("Ua%d" % i, [128, 1039], F32, es1) for i in range(2)]
                Us = [sb("Us%d" % i, [128, 16, 19], F32, es1) for i in range(2)]
                Usa = [sb("Usa%d" % i, [128, 16, 19], F32, es1) for i in range(2)]
                dtmp = sb("dtmp", [128, 1024], F32, es1)
                dtmps = sb("dtmps", [128, 16, 4], F32, es1)
                kb.op("pool", lambda e: e.memset(dT[:], 0.0), writes=[dT])
                kb.dma("sp", ivc[:, :, :].rearrange("p g t -> p (g t)"), invc.a[0, :].partition_broadcast(128), writes=[ivc])
                kb.dma("pool", wp[:], wpool.a.rearrange("g (c p) e -> p (g c) e", p=128), writes=[wp])
                kb.dma("sp", psc[:], pscale.a[:, :], writes=[psc])
                for j in range(8):
                    g = j // 2
                    U, UsJ = Up[j % 2], Us[j % 2]
                    kb.dma("sp", U[:], uTp.a[j * 128:(j + 1) * 128, :], writes=[U])
                    kb.dma("sp", UsJ[:], uTs.a[j * 128:(j + 1) * 128, :, :], writes=[UsJ])
                    cur, curs = U, UsJ
                    lo = 0
                    for k in range(g + 1):
                        sh = 1 << k
                        nxt, nxts = Ua[k % 2], Usa[k % 2]
                        l2 = lo + sh
                        kb.op("dve", lambda e: e.tensor_tensor(out=nxt[:, l2:1039], in0=cur[:, l2:1039],
                                                               in1=cur[:, lo:1039 - sh], op=ALU.add),
                              reads=[cur], writes=[nxt])
                        kb.op("pool", lambda e: e.tensor_tensor(out=nxts[:, :, l2:19], in0=curs[:, :, l2:19],
                                                                in1=curs[:, :, lo:19 - sh], op=ALU.add),
                              reads=[curs], writes=[nxts])
                        cur, curs = nxt, nxts
                        lo = l2
                    w = float(1 << (g + 1))
                    kb.op("dve", lambda e: e.tensor_tensor(out=dtmp[:], in0=cur[:, 15:1039], in1=ivc[:, g, :], op=ALU.mult),
                          reads=[cur, ivc], writes=[dtmp])
                    kb.op("dve", lambda e: e.tensor_tensor(out=dT[:, j, 0:1024], in0=dtmp[:], in1=U[:, 15:1039],
                                                           op=ALU.subtract), reads=[dtmp, U], writes=[dT])
                    kb.op("pool", lambda e: e.tensor_scalar(out=dtmps[:], in0=curs[:, :, 15:19], scalar1=1.0 / w,
                                                            scalar2=None, op0=ALU.mult), reads=[curs], writes=[dtmps])
                    kb.op("pool", lambda e: e.tensor_tensor(
                        out=dT[:, j, 1024:1088].rearrange("p (b t) -> p b t", t=4), in0=dtmps[:],
                        in1=UsJ[:, :, 15:19], op=ALU.subtract), reads=[dtmps, UsJ], writes=[dT])
                pp = [kb.ps("pp%d" % i, i, 0, 512) for i in range(2)]
                it = 0
                for je in range(8):
                    g, el = je // 2, je % 2
                    for (t0, tn) in TGS:
                        P = pp[it % 2]
                        it += 1
                        for cc in range(2):
                            kb.op("pe", lambda e: e.matmul(P[:, 0:tn], lhsT=wp[:, g * 2 + cc, el * 128:(el + 1) * 128],
                                                           rhs=dT[:, g * 2 + cc, t0:t0 + tn], start=(cc == 0), stop=(cc == 1)),
                                  reads=[wp, dT], writes=[P])
                        kb.op("act", lambda e: e.activation(out=mixT[:, 8 + je, t0:t0 + tn], in_=P[:, 0:tn], func=AF.Copy,
                                                            scale=psc[:, je:je + 1]), reads=[P, psc], writes=[mixT])
                _barrier(kb, allb)
            with ExitStack() as es2:
                wo = [sb("wo%d" % i, [128, 16, 512], BF16, es2) for i in range(2)]
                xs = [sb("xs%d" % i, [128, 512], F32, es2) for i in range(3)]
                zp = [kb.ps("zq%d" % i, 2 + i, 0, 512) for i in range(2)]
                it = 0
                for n in range(4):
                    W = wo[n % 2]
                    kb.dma("pool", W[:], wout.a[:, n * 512:(n + 1) * 512].rearrange("(c p) n -> p c n", p=128), writes=[W])
                    for i in range(NTB):
                        X, Z = xs[it % 3], zp[it % 2]
                        it += 1
                        kb.dma("sp", X[:], xc.a[i * 128:(i + 1) * 128, n * 512:(n + 1) * 512], writes=[X])
                        for c in range(16):
                            kb.op("pe", lambda e: e.matmul(Z[:], lhsT=mixT[:, c, i * 128:(i + 1) * 128], rhs=W[:, c, :],
                                                           start=(c == 0), stop=(c == 15)), reads=[mixT, W], writes=[Z])
                        kb.op("dve", lambda e: e.tensor_tensor(out=Y[:, i, n * 512:(n + 1) * 512], in0=Z[:], in1=X[:],
                                                               op=ALU.add), reads=[Z, X], writes=[Y])
                _barrier(kb, allb)
        h2T = sb("h2T", [128, 16, TB_ROWS], BF16)
        with ExitStack() as es3:
            gf = sb("gf", [128, D], F32, es3)
            brb = sb("brb", [128, 20], F32, es3)
            wrf = sb("wrf", [128, 16, 20], F32, es3)
            whi = sb("whi", [128, 16, 20], BF16, es3)
            wlo = sb("wlo", [128, 16, 20], BF16, es3)
            junk = sb("junk2", [128, D], BF16, es3)
            h2f = sb("h2f", [128, D], F32, es3)
            hhi = sb("hhi", [128, D], BF16, es3)
            hlo = sb("hlo", [128, D], BF16, es3)
            loT = sb("loT", [128, 16, 128], BF16, es3)
            st = sb("st3", [128, 8], F32, es3)
            lg = sb("lg", [128, 20], F32, es3)
            r_ = sb("rt", [128, 48], F32, es3)
            tpa = [kb.ps("tpa%d" % i, 4 + i, 0, 512, BF16, split=8) for i in range(2)]
            lgp = kb.ps("lgp", 6, 0, 20)
            kb.dma("sp", gf[:], gffn.a[0, :].partition_broadcast(128), writes=[gf])
            kb.dma("sp", brb[:], br.a[0, :].partition_broadcast(128), writes=[brb])
            kb.dma("sp", wrf[:], wr.a.rearrange("(c p) n -> p c n", p=128), writes=[wrf])
            kb.op("act", lambda e: e.copy(out=whi[:], in_=wrf[:]), reads=[wrf], writes=[whi])
            kb.op("dve", lambda e: e.tensor_tensor(out=wlo[:], in0=wrf[:], in1=whi[:], op=ALU.subtract),
                  reads=[wrf, whi], writes=[wlo])
            for i in range(NTB):
                ts = slice(i * 128, (i + 1) * 128)
                kb.op("act", lambda e: e.activation(out=junk[:], in_=Y[:, i, :], func=AF.Square, accum_out=st[:, 0:1]),
                      reads=[Y], writes=[junk, st])
                kb.op("act", lambda e: e.activation(out=st[:, 1:2], in_=st[:, 0:1], func=AF.Sqrt, scale=1.0 / D, bias=EPS),
                      reads=[st], writes=[st])
                kb.op("dve", lambda e: e.reciprocal(out=st[:, 2:3], in_=st[:, 1:2]), reads=[st], writes=[st])
                kb.op("dve", lambda e: e.scalar_tensor_tensor(out=h2f[:], in0=Y[:, i, :], scalar=st[:, 2:3], in1=gf[:],
                                                              op0=ALU.mult, op1=ALU.mult), reads=[Y, st, gf], writes=[h2f])
                kb.op("act", lambda e: e.copy(out=hhi[:], in_=h2f[:]), reads=[h2f], writes=[hhi])
                kb.op("dve", lambda e: e.tensor_tensor(out=hlo[:], in0=h2f[:], in1=hhi[:], op=ALU.subtract),
                      reads=[h2f, hhi], writes=[hlo])
                for (src, dst, off) in ((hhi, h2T, i * 128), (hlo, loT, 0)):
                    for half in range(2):
                        T = tpa[half]
                        for cc in range(8):
                            c = half * 8 + cc
                            kb.op("pe", lambda e: e.transpose(out=T[:, cc, :], in_=src[:, c * 128:(c + 1) * 128],
                                                              identity=identb[:]), reads=[src, identb], writes=[T])
                        kb.op("act", lambda e: e.copy(out=dst[:, half * 8:(half + 1) * 8, off:off + 128], in_=T[:]),
                              reads=[T], writes=[dst])
                k = 0
                for c in range(16):
                    for (a, b) in ((h2T[:, c, ts], whi), (h2T[:, c, ts], wlo), (loT[:, c, :], whi)):
                        kb.op("pe", lambda e: e.matmul(lgp[:], lhsT=a, rhs=b[:, c, :], start=(k == 0), stop=(k == 47)),
                              reads=[h2T, loT, whi, wlo], writes=[lgp])
                        k += 1
                kb.op("dve", lambda e: e.tensor_tensor(out=lg[:], in0=lgp[:], in1=brb[:], op=ALU.add),
                      reads=[lgp, brb], writes=[lg])
                R = r_
                gl = lg[:, 0:4]
                el3 = lg[:, 4:20].rearrange("p (g e) -> p g e", g=4)
                def dv(fn, reads, writes):
                    kb.op("dve", fn, reads=reads, writes=writes)
                dv(lambda e: e.reduce_max(out=R[:, 0:1], in_=gl, axis=AX.X), [lg], [R])
                dv(lambda e: e.tensor_scalar(out=R[:, 4:8], in0=gl, scalar1=R[:, 0:1], scalar2=None, op0=ALU.subtract), [lg, R], [R])
                kb.op("act", lambda e: e.activation(out=R[:, 8:12], in_=R[:, 4:8], func=AF.Exp, accum_out=R[:, 1:2]),
                      reads=[R], writes=[R])
                dv(lambda e: e.reciprocal(out=R[:, 2:3], in_=R[:, 1:2]), [R], [R])
                dv(lambda e: e.tensor_scalar(out=R[:, 12:16], in0=gl, scalar1=R[:, 0:1], scalar2=None, op0=ALU.is_ge), [lg, R], [R])
                dv(lambda e: e.tensor_tensor(out=R[:, 16:32].rearrange("p (g e) -> p g e", g=4), in0=el3,
                                             in1=R[:, 12:16].unsqueeze(2).to_broadcast([128, 4, 4]), op=ALU.mult), [lg, R], [R])
                dv(lambda e: e.tensor_reduce(out=R[:, 32:36], in_=R[:, 16:32].rearrange("p (g e) -> p e g", g=4),
                                             axis=AX.X, op=ALU.add), [R], [R])
                dv(lambda e: e.reduce_max(out=R[:, 36:37], in_=R[:, 32:36], axis=AX.X), [R], [R])
                dv(lambda e: e.tensor_scalar(out=R[:, 40:44], in0=R[:, 32:36], scalar1=R[:, 36:37], scalar2=NEG,
                                             op0=ALU.is_ge, op1=ALU.mult), [R], [R])
                dv(lambda e: e.tensor_tensor(out=R[:, 40:44], in0=R[:, 40:44], in1=R[:, 32:36], op=ALU.add), [R], [R])
                dv(lambda e: e.reduce_max(out=R[:, 37:38], in_=R[:, 40:44], axis=AX.X), [R], [R])
                dv(lambda e: e.tensor_scalar(out=R[:, 44:48], in0=R[:, 32:36], scalar1=R[:, 37:38], scalar2=None,
                                             op0=ALU.is_ge), [R], [R])
                dv(lambda e: e.tensor_scalar(out=R[:, 40:44], in0=R[:, 32:36], scalar1=R[:, 36:37], scalar2=None,
                                             op0=ALU.subtract), [R], [R])
                kb.op("act", lambda e: e.activation(out=R[:, 40:44], in_=R[:, 40:44], func=AF.Exp), reads=[R], writes=[R])
                dv(lambda e: e.tensor_tensor(out=R[:, 40:44], in0=R[:, 40:44], in1=R[:, 44:48], op=ALU.mult), [R], [R])
                dv(lambda e: e.reduce_sum(out=R[:, 38:39], in_=R[:, 40:44], axis=AX.X), [R], [R])
                dv(lambda e: e.reciprocal(out=R[:, 39:40], in_=R[:, 38:39]), [R], [R])
                dv(lambda e: e.tensor_scalar(out=R[:, 40:44], in0=R[:, 40:44], scalar1=R[:, 39:40], scalar2=R[:, 2:3],
                                             op0=ALU.mult, op1=ALU.mult), [R], [R])
                dv(lambda e: e.tensor_tensor(out=comb[:, i, :].rearrange("p (g e) -> p g e", g=4),
                                             in0=R[:, 12:16].unsqueeze(2).to_broadcast([128, 4, 4]),
                                             in1=R[:, 40:44].unsqueeze(1).to_broadcast([128, 4, 4]), op=ALU.mult), [R], [comb])
            _barrier(kb, allb)
        HF = DF // 2
        wg = [sb("wg%d" % i, [128, 16, HF], BF16) for i in range(2)]
        wu = [sb("wu%d" % i, [128, 16, HF], BF16) for i in range(2)]
        wd = [sb("wd%d" % i, [128, 3, D], BF16) for i in range(2)]
        actT = [sb("actT%d" % i, [128, 3, 512], BF16) for i in range(2)]
        sA = [sb("sA%d" % i, [128, 512], BF16) for i in range(2)]
        pa = [kb.ps("pa%d" % i, i, 0, 512) for i in range(2)]
        pb = [kb.ps("pb%d" % i, 2 + i, 0, 512) for i in range(2)]
        pd = [kb.ps("pd%d" % i, 4 + i, 0, 512) for i in range(2)]
        ia = idn = iu = ig = 0
        for ex in range(ne if do_moe else 0):
            for hf in range(2):
                WG, WU, WD = wg[iu % 2], wu[iu % 2], wd[iu % 2]
                iu += 1
                kb.dma("pool", WG[:], wgate.a[ex, :, hf * HF:(hf + 1) * HF].rearrange("(c p) f -> p c f", p=128), writes=[WG])
                kb.dma("pool", WU[:], wup.a[ex, :, hf * HF:(hf + 1) * HF].rearrange("(c p) f -> p c f", p=128), writes=[WU])
                kb.dma("pool", WD[:], wdown.a[ex, hf * HF:(hf + 1) * HF, :].rearrange("(c p) n -> p c n", p=128), writes=[WD])
                for gi, (t0, tn) in enumerate(TGS):
                    AT = actT[ig % 2]
                    ig += 1
                    for fc in range(3):
                        PA, PB_, SA = pa[ia % 2], pb[ia % 2], sA[ia % 2]
                        ia += 1
                        for c in range(16):
                            kb.op("pe", lambda e: e.matmul(PA[:, 0:tn], lhsT=WG[:, c, fc * 128:(fc + 1) * 128],
                                                           rhs=h2T[:, c, t0:t0 + tn], start=(c == 0), stop=(c == 15)),
                                  reads=[WG, h2T], writes=[PA])
                        for c in range(16):
                            kb.op("pe", lambda e: e.matmul(PB_[:, 0:tn], lhsT=WU[:, c, fc * 128:(fc + 1) * 128],
                                                           rhs=h2T[:, c, t0:t0 + tn], start=(c == 0), stop=(c == 15)),
                                  reads=[WU, h2T], writes=[PB_])
                        kb.op("act", lambda e: e.activation(out=SA[:, 0:tn], in_=PA[:, 0:tn], func=AF.Silu),
                              reads=[PA], writes=[SA])
                        kb.op("dve", lambda e: e.tensor_tensor(out=AT[:, fc, 0:tn], in0=PB_[:, 0:tn], in1=SA[:, 0:tn],
                                                               op=ALU.mult), reads=[PB_, SA], writes=[AT])
                    for ii in range(tn // 128):
                        i = t0 // 128 + ii
                        for n in range(4):
                            PD = pd[idn % 2]
                            idn += 1
                            for fc in range(3):
                                kb.op("pe", lambda e: e.matmul(PD[:], lhsT=AT[:, fc, ii * 128:(ii + 1) * 128],
                                                               rhs=WD[:, fc, n * 512:(n + 1) * 512], start=(fc == 0), stop=(fc == 2)),
                                      reads=[AT, WD], writes=[PD])
                            kb.op("dve", lambda e: e.scalar_tensor_tensor(
                                out=Y[:, i, n * 512:(n + 1) * 512], in0=PD[:], scalar=comb[:, i, ex:ex + 1],
                                in1=Y[:, i, n * 512:(n + 1) * 512], op0=ALU.mult, op1=ALU.add),
                                reads=[PD, comb, Y], writes=[Y])
        for i in range(NTB):
            kb.dma("sp", y.a[i * 128:(i + 1) * 128, :], Y[:, i, :], reads=[Y], final=True)
        kb.finish()
    return nc


def run_attn(inputs):
    x = np.concatenate([np.asarray(inputs["x_prompt"], np.float32).reshape(S, D),
                        np.asarray(inputs["x_sample"], np.float32).reshape(NS, D)], 0)
    w_in = np.asarray(inputs["w_in"], np.float32)[0]
    ck = np.asarray(inputs["cache_k"], np.float32)[0]
    cv = np.asarray(inputs["cache_v"], np.float32)[0]
    pt = np.ascontiguousarray(np.asarray(inputs["page_table"], np.int32).reshape(-1, 1))
    gmix = np.asarray(inputs["g_mix"], np.float32).reshape(1, D)
    gq = np.asarray(inputs["g_q"], np.float32).reshape(1, HD)
    gk = np.asarray(inputs["g_k"], np.float32).reshape(1, HD)
    in_maps = []
    for h in range(NH):
        cols = np.concatenate([np.arange(j * 1024 + h * HD, j * 1024 + (h + 1) * HD) for j in range(4)])
        in_maps.append({
            "x": x, "wh": np.ascontiguousarray(w_in[:, cols]), "gmix": gmix, "gq": gq, "gk": gk,
            "kc": np.ascontiguousarray(ck[:, :, h, :].transpose(0, 2, 1)).reshape(NPHYS, HD * 128),
            "vc": np.ascontiguousarray(cv[:, :, h, :]).reshape(NPHYS, 128 * HD),
            "pt": pt,
        })
    nc = build_attn()
    res = run_bass_kernel_spmd(nc, in_maps, core_ids=list(range(8)))
    kvu = np.stack([np.asarray(r["kvu"]) for r in res.results], 0)
    oo = np.stack([np.asarray(r["oo"]) for r in res.results], 0)
    return kvu, oo


def run_ffn(inputs, kvu, oo):
    POOLW = (2, 4, 8, 16)
    xp = np.asarray(inputs["x_prompt"], np.float32).reshape(S, D)
    xs = np.asarray(inputs["x_sample"], np.float32).reshape(NS, D)
    sp = np.asarray(inputs["state_pool"], np.float32)[0]
    u = np.ascontiguousarray(kvu[:, :, 2, :].transpose(1, 0, 2)).reshape(NTOK, 1024)
    oa = np.ascontiguousarray(oo.transpose(1, 0, 2)).reshape(NTOK, 1024)
    wr = np.ascontiguousarray(np.concatenate([np.asarray(inputs["w_group_router"], np.float32)[0],
                                              np.asarray(inputs["w_expert_router"], np.float32)[0].reshape(D, 16)], 1))
    br = np.concatenate([np.asarray(inputs["b_group_router"], np.float32)[0].reshape(1, 4),
                         np.asarray(inputs["b_expert_router"], np.float32)[0].reshape(1, 16)], 1)
    common = {
        "wpool": np.asarray(inputs["w_pool"], np.float32)[0],
        "pscale": np.ascontiguousarray(np.asarray(inputs["pool_scale"], np.float32)[0].reshape(8, 128).T),
        "wout": np.asarray(inputs["w_out"], np.float32)[0],
        "gffn": np.asarray(inputs["g_ffn"], np.float32).reshape(1, D),
        "wr": wr, "br": np.ascontiguousarray(br),
        "wgate": np.asarray(inputs["w_gate"], np.float32)[0],
        "wup": np.asarray(inputs["w_up"], np.float32)[0],
        "wdown": np.asarray(inputs["w_down"], np.float32)[0],
    }
    in_maps = []
    for c in range(8):
        prow = np.arange(1024 * c, 1024 * (c + 1))
        srow = S + np.arange(64 * c, 64 * (c + 1))
        xc = np.zeros((TB_ROWS, D), np.float32)
        xc[:1024] = xp[prow]
        xc[1024:1088] = xs[64 * c:64 * (c + 1)]
        oaT = np.zeros((1024, TB_ROWS), np.float32)
        oaT[:, :1024] = oa[prow].T
        oaT[:, 1024:1088] = oa[srow].T
        uTp = np.zeros((1024, 1039), np.float32)
        uTp[:, 15:] = u[prow].T
        if c > 0:
            uTp[:, :15] = u[1024 * c - 15:1024 * c].T
        uTs = np.zeros((1024, 16, 19), np.float32)
        uTs[:, :, :15] = sp[16 * c:16 * (c + 1)].transpose(2, 0, 1)
        uTs[:, :, 15:] = u[srow].reshape(16, 4, 1024).transpose(2, 0, 1)
        pos = np.arange(1024 * c, 1024 * (c + 1))
        invc = np.stack([1.0 / np.minimum(float(w), pos + 1.0) for w in POOLW], 0).astype(np.float32).reshape(1, 4096)
        m = dict(common)
        m.update({"xc": xc, "oaT": oaT, "uTp": uTp, "uTs": uTs, "invc": invc,
                  "spool": np.ascontiguousarray(sp[16 * c:16 * (c + 1), 4:15, :]).reshape(176, 1024)})
        in_maps.append(m)
    nc = build_ffn()
    res = run_bass_kernel_spmd(nc, in_maps, core_ids=list(range(8)))
    ys = [np.asarray(r["y"]) for r in res.results]
    pso = [np.asarray(r["pso"]).reshape(16, 11, 1024) for r in res.results]
    return ys, pso, u


def kernel(**inputs):
    kvu, oo = run_attn(inputs)
    ys, pso, u = run_ffn(inputs, kvu, oo)
    f = np.float32
    y_prompt = np.concatenate([y[:1024] for y in ys], 0).reshape(1, S, D).astype(f)
    y_sample = np.concatenate([y[1024:1088] for y in ys], 0).reshape(NSEQ, TS, D).astype(f)
    k_all = np.ascontiguousarray(kvu[:, :, 0, :].transpose(1, 0, 2))
    v_all = np.ascontiguousarray(kvu[:, :, 1, :].transpose(1, 0, 2))
    k_prompt = k_all[:S].reshape(1, 1, S, NH, HD).astype(f)
    v_prompt = v_all[:S].reshape(1, 1, S, NH, HD).astype(f)
    k_sample = k_all[S:].reshape(1, NSEQ, TS, NH, HD).astype(f)
    v_sample = v_all[S:].reshape(1, NSEQ, TS, NH, HD).astype(f)
    pool_prompt = u[S - 15:S].reshape(1, 1, 15, 1024).astype(f)
    pool_sample = np.concatenate([np.concatenate(pso, 0), u[S:].reshape(NSEQ, TS, 1024)], 1).reshape(1, NSEQ, 15, 1024).astype(f)
    return (y_prompt, y_sample, k_prompt, v_prompt, pool_prompt, k_sample, v_sample, pool_sample)
```

```python
import numpy as np
from contextlib import ExitStack
import concourse.bass as bass
import concourse.mybir as mybir
from concourse.bass_utils import run_bass_kernel_spmd

F32 = mybir.dt.float32
BF16 = mybir.dt.bfloat16
I32 = mybir.dt.int32
ALU = mybir.AluOpType
AF = mybir.ActivationFunctionType
AX = mybir.AxisListType

D = 2048
S = 8192
NSEQ = 128
TS = 4
NS = NSEQ * TS
NTOK = S + NS
NT = NTOK // 128
HD = 128
NH = 8
NPAGES = 16
NPHYS = 2560
EPS = 1e-6
SCALE = HD ** -0.5
NEG = -1.0e30


class _St:
    def __init__(self):
        self.w = None
        self.r = {}


class TB:
    def __init__(self, a, name, st=None):
        self.a = a
        self.name = name
        self.s = st if st is not None else _St()
        self.dsem = None
        self.dcnt = 0

    @property
    def w(self):
        return self.s.w

    @w.setter
    def w(self, v):
        self.s.w = v

    @property
    def r(self):
        return self.s.r

    @r.setter
    def r(self, v):
        self.s.r = v

    def __getitem__(self, idx):
        return self.a[idx]


class Tag:
    __slots__ = ("key", "sem", "val", "buf")

    def __init__(self, key, sem, val, buf=None):
        self.key, self.sem, self.val, self.buf = key, sem, val, buf


class KB:
    def __init__(self, nc, es):
        self.nc, self.es = nc, es
        self.E = {"pe": nc.tensor, "act": nc.scalar, "dve": nc.vector, "pool": nc.gpsimd, "sp": nc.sync}
        self.sem = {k: es.enter_context(nc.semaphore("sem_" + k)) for k in self.E}
        self.cnt = {k: 0 for k in self.E}
        self.seen = {k: {} for k in self.E}
        self.outs = []
        self.n = 0

    def sb(self, name, shape, dt):
        return TB(self.es.enter_context(self.nc.sbuf_tensor(name, shape, dt)), name)

    def banks(self):
        self.bk = [self.es.enter_context(self.nc.psum_tensor("bank%d" % i, [128, 512], F32)) for i in range(8)]
        self.bst = [_St() for i in range(8)]

    def ps(self, name, bank, off, n, dt=F32, split=None, parts=128):
        a = self.bk[bank][0:parts, off:off + n]
        if dt == BF16:
            a = a.bitcast(BF16)
        if split is not None:
            a = a.rearrange("p (a b) -> p a b", a=split)
        return TB(a, name, self.bst[bank])

    def dr(self, name, shape, dt, kind):
        return TB(self.nc.dram_tensor(name, shape, dt, kind=kind).ap(), name)

    def _wait(self, e, reads, writes):
        tags = []
        for b in reads:
            if b.w is not None:
                tags.append(b.w)
        for b in writes:
            if b.w is not None:
                tags.append(b.w)
            tags.extend(b.r.values())
        eng = self.E[e]
        for t in tags:
            if e == "pe" and t.key == "pe":
                continue
            val = t.buf.dcnt if t.buf is not None else t.val
            if self.seen[e].get(t.key, 0) < val:
                eng.wait_ge(t.sem, val)
                self.seen[e][t.key] = val

    def op(self, e, fn, reads=(), writes=()):
        self._wait(e, reads, writes)
        ins = fn(self.E[e])
        self.cnt[e] += 1
        ins.then_inc(self.sem[e], 1)
        tag = Tag(e, self.sem[e], self.cnt[e])
        for b in reads:
            b.r[e] = tag
        for b in writes:
            b.w = tag
            b.r = {}
        return ins

    def _dma_done(self, ins, reads, writes, final):
        b = writes[0] if writes else reads[0]
        if b.dsem is None:
            self.n += 1
            b.dsem = self.es.enter_context(self.nc.semaphore("dsem%d" % self.n))
        b.dcnt += 16
        ins.then_inc(b.dsem, 16)
        tag = Tag("d_" + b.name, b.dsem, b.dcnt, b)
        for x in reads:
            x.r[tag.key] = tag
        for x in writes:
            x.w = tag
            x.r = {}
        if final and b not in self.outs:
            self.outs.append(b)

    def dma(self, q, out, in_, reads=(), writes=(), final=False):
        self._wait(q, reads, writes)
        ins = self.E[q].dma_start(out=out, in_=in_)
        self._dma_done(ins, list(reads), list(writes), final)

    def gather(self, out, in_, idx_ap, reads=(), writes=()):
        self._wait("pool", reads, writes)
        ins = self.nc.gpsimd.indirect_dma_start(
            out=out, out_offset=None, in_=in_,
            in_offset=bass.IndirectOffsetOnAxis(ap=idx_ap, axis=0))
        self._dma_done(ins, list(reads), list(writes), False)

    def finish(self):
        for b in self.outs:
            self.nc.sync.wait_ge(b.dsem, b.dcnt)
        for e in self.E:
            if e != "sp" and self.cnt[e] > 0:
                self.nc.sync.wait_ge(self.sem[e], self.cnt[e])


def _consts(kb):
    nc = kb.nc
    onesf = kb.sb("onesf", [128, 128], F32)
    identf = kb.sb("identf", [128, 128], F32)
    identb = kb.sb("identb", [128, 128], BF16)
    trib = kb.sb("trib", [128, 128], BF16)
    kb.op("pool", lambda e: e.memset(onesf[:], 1.0), writes=[onesf])
    kb.op("pool", lambda e: e.affine_select(out=identf[:], in_=onesf[:], pattern=[[1, 128]],
                                            compare_op=ALU.is_equal, fill=0.0, base=0,
                                            channel_multiplier=-1), reads=[onesf], writes=[identf])
    kb.op("pool", lambda e: e.tensor_copy(out=identb[:], in_=identf[:]), reads=[identf], writes=[identb])
    kb.op("pool", lambda e: e.affine_select(out=trib[:], in_=onesf[:], pattern=[[1, 128]],
                                            compare_op=ALU.is_ge, fill=0.0, base=0,
                                            channel_multiplier=-1), reads=[onesf], writes=[trib])
    return onesf, identf, identb, trib


def build_attn(nphys=NPHYS, ngroups=16, tiles=None, nchunks=64, nseq=NSEQ, xrows=NTOK, stop=0, sub=9):
    nc = bass.Bass("TRN2", target_bir_lowering=False)
    es = ExitStack()
    with es:
        kb = KB(nc, es)
        kb.banks()
        x = kb.dr("x", [xrows, D], F32, "ExternalInput")
        wh = kb.dr("wh", [D, 512], F32, "ExternalInput")
        gmix = kb.dr("gmix", [1, D], F32, "ExternalInput")
        gq = kb.dr("gq", [1, HD], F32, "ExternalInput")
        gk = kb.dr("gk", [1, HD], F32, "ExternalInput")
        kc = kb.dr("kc", [nphys, HD * 128], F32, "ExternalInput")
        vc = kb.dr("vc", [nphys, 128 * HD], F32, "ExternalInput")
        pt = kb.dr("pt", [NSEQ * NPAGES, 1], I32, "ExternalInput")
        kvu = kb.dr("kvu", [NTOK, 3, HD], F32, "ExternalOutput")
        oo = kb.dr("oo", [NTOK, HD], F32, "ExternalOutput")
        scrK = [kb.dr("scrK%d" % g, [128, HD * 128], F32, "Internal") for g in range(16)]
        scrV = [kb.dr("scrV%d" % g, [128, HD * 128], F32, "Internal") for g in range(16)]
        kvu_s = TB(kvu.a, "kvu_s")

        onesf, identf, identb, trib = _consts(kb)

        ptt = kb.sb("ptt", [128, 16], I32)
        ptt2 = kb.sb("ptt2", [128, 16, 2], I32)
        stage = kb.sb("stage", [128, HD * 64], F32)
        for g in range(16):
            kb.dma("sp", ptt[:, g:g + 1], pt.a[g * 128:(g + 1) * 128, :], writes=[ptt])
        kb.op("dve", lambda e: e.tensor_scalar(out=ptt2[:, :, 0], in0=ptt[:], scalar1=2.0, scalar2=None,
                                               op0=ALU.mult), reads=[ptt], writes=[ptt2])
        kb.op("dve", lambda e: e.tensor_scalar(out=ptt2[:, :, 1], in0=ptt[:], scalar1=2.0, scalar2=1.0,
                                               op0=ALU.mult, op1=ALU.add), reads=[ptt], writes=[ptt2])
        kc2 = kc.a.rearrange("n (h e) -> (n h) e", h=2)
        vc2 = vc.a.rearrange("n (h e) -> (n h) e", h=2)
        for g in range(ngroups):
            for (src, scr) in ((kc2, scrK[g]), (vc2, scrV[g])):
                for hf in range(2):
                    kb.gather(stage[:], src, ptt2[:, g, hf:hf + 1], reads=[ptt2], writes=[stage])
                    kb.dma("sp", scr.a[:, hf * HD * 64:(hf + 1) * HD * 64], stage[:], reads=[stage], writes=[scr])

        gbc = kb.sb("gbc", [128, D], F32)
        gqb = kb.sb("gqb", [128, HD], F32)
        gkb = kb.sb("gkb", [128, HD], F32)
        wb = kb.sb("wb", [128, 16, 512], BF16)
        kb.dma("sp", gbc[:], gmix.a[0, :].partition_broadcast(128), writes=[gbc])
        kb.dma("sp", gqb[:], gq.a[0, :].partition_broadcast(128), writes=[gqb])
        kb.dma("sp", gkb[:], gk.a[0, :].partition_broadcast(128), writes=[gkb])
        kb.dma("pool", wb[:], wh.a.rearrange("(c p) n -> p c n", p=128), writes=[wb])

        if stop == 1:
            kb.dma("sp", oo.a[0:128, :], onesf[:], reads=[onesf], final=True)
            kb.finish()
            return nc
        QT = kb.sb("QT", [128, NTOK], BF16)
        KT = kb.sb("KT", [128, NTOK], BF16)
        Vx = kb.sb("Vx", [128, NT, HD + 1], BF16)
        ksum = kb.sb("ksum", [128, 64], F32)
        kmT = kb.sb("kmT", [128, 32], BF16)
        kb.op("pool", lambda e: e.memset(Vx[:, :, HD:HD + 1], 1.0), writes=[Vx])
        kb.op("pool", lambda e: e.memset(ksum[:], 0.0), writes=[ksum])

        xt = [kb.sb("xt%d" % i, [128, D], F32) for i in range(2)]
        junk = kb.sb("junk", [128, D], BF16)
        hn = kb.sb("hn", [128, D], BF16)
        hT = [kb.sb("hT%d" % i, [128, 16, 128], BF16) for i in range(2)]
        st = [kb.sb("st%d" % i, [128, 8], F32) for i in range(2)]
        o3 = [kb.sb("o3%d" % i, [128, 3, HD], F32) for i in range(2)]
        qn = [kb.sb("qn%d" % i, [128, 2, HD], BF16) for i in range(2)]
        zp = [kb.ps("zp%d" % i, i, 0, 512) for i in range(2)]
        tp = [kb.ps("tp%d" % i, 2 + i, 0, 512, BF16, split=8) for i in range(2)]
        tq = kb.ps("tq", 4, 0, 128, BF16, split=2)

        tl = list(range(NT) if tiles is None else tiles)
        if tl:
            kb.dma("sp", xt[tl[0] % 2][:], x.a[tl[0] * 128:(tl[0] + 1) * 128, :], writes=[xt[tl[0] % 2]])
        for ti, i in enumerate(tl):
            X, Hh, Sx, O3, Qn, Z = xt[i % 2], hT[i % 2], st[i % 2], o3[i % 2], qn[i % 2], zp[i % 2]
            if ti + 1 < len(tl):
                i1 = tl[ti + 1]
                kb.dma("sp", xt[i1 % 2][:], x.a[i1 * 128:(i1 + 1) * 128, :], writes=[xt[i1 % 2]])
            kb.op("act", lambda e: e.activation(out=junk[:], in_=X[:], func=AF.Square,
                                                accum_out=Sx[:, 0:1]), reads=[X], writes=[junk, Sx])
            kb.op("act", lambda e: e.activation(out=Sx[:, 1:2], in_=Sx[:, 0:1], func=AF.Sqrt,
                                                scale=1.0 / D, bias=EPS), reads=[Sx], writes=[Sx])
            kb.op("dve", lambda e: e.reciprocal(out=Sx[:, 2:3], in_=Sx[:, 1:2]), reads=[Sx], writes=[Sx])
            kb.op("dve", lambda e: e.scalar_tensor_tensor(out=hn[:], in0=X[:], scalar=Sx[:, 2:3], in1=gbc[:],
                                                          op0=ALU.mult, op1=ALU.mult),
                  reads=[X, Sx, gbc], writes=[hn])
            if stop == 2:
                kb.dma("sp", oo.a[0:128, :], onesf[:], reads=[onesf], final=True); kb.finish(); return nc
            for half in range(2):
                T = tp[half]
                for cc in range(8):
                    c = half * 8 + cc
                    kb.op("pe", lambda e: e.transpose(out=T[:, cc, :], in_=hn[:, c * 128:(c + 1) * 128],
                                                      identity=identb[:]), reads=[hn, identb], writes=[T])
                if half == 0:
                    kb.op("act", lambda e: e.copy(out=Hh[:, 0:8, :], in_=T[:]), reads=[T], writes=[Hh])
                else:
                    kb.op("dve", lambda e: e.tensor_copy(out=Hh[:, 8:16, :], in_=T[:]), reads=[T], writes=[Hh])
            if stop == 3:
                kb.dma("sp", oo.a[0:128, :], onesf[:], reads=[onesf], final=True); kb.finish(); return nc
            for c in range(16):
                kb.op("pe", lambda e: e.matmul(Z[:], lhsT=Hh[:, c, :], rhs=wb[:, c, :],
                                               start=(c == 0), stop=(c == 15)), reads=[Hh, wb], writes=[Z])
            if stop == 4:
                kb.dma("sp", oo.a[0:128, :], onesf[:], reads=[onesf], final=True); kb.finish(); return nc
            kb.op("act", lambda e: e.activation(out=junk[:, 0:128], in_=Z[:, 0:128], func=AF.Square,
                                                accum_out=Sx[:, 3:4]), reads=[Z], writes=[junk, Sx])
            kb.op("act", lambda e: e.activation(out=junk[:, 128:256], in_=Z[:, 128:256], func=AF.Square,
                                                accum_out=Sx[:, 4:5]), reads=[Z], writes=[junk, Sx])
            kb.op("act", lambda e: e.activation(out=Sx[:, 3:5], in_=Sx[:, 3:5], func=AF.Sqrt,
                                                scale=1.0 / HD, bias=EPS), reads=[Sx], writes=[Sx])
            kb.op("dve", lambda e: e.reciprocal(out=Sx[:, 5:7], in_=Sx[:, 3:5]), reads=[Sx], writes=[Sx])
            kb.op("dve", lambda e: e.scalar_tensor_tensor(out=Qn[:, 0, :], in0=Z[:, 0:128], scalar=Sx[:, 5:6], in1=gqb[:],
                                                          op0=ALU.mult, op1=ALU.mult),
                  reads=[Z, Sx, gqb], writes=[Qn])
            kb.op("dve", lambda e: e.scalar_tensor_tensor(out=O3[:, 0, :], in0=Z[:, 128:256], scalar=Sx[:, 6:7],
                                                          in1=gkb[:], op0=ALU.mult, op1=ALU.mult),
                  reads=[Z, Sx, gkb], writes=[O3])
            kb.op("act", lambda e: e.copy(out=O3[:, 1:3, :], in_=Z[:, 256:512].rearrange("p (a b) -> p a b", a=2)),
                  reads=[Z], writes=[O3])
            kb.op("pool", lambda e: e.tensor_copy(out=Vx[:, i, 0:HD], in_=O3[:, 1, :]), reads=[O3], writes=[Vx])
            if i < 64:
                kb.dma("sp", kvu.a[i * 128:(i + 1) * 128, :, :], O3[:], reads=[O3], final=True)
            else:
                kb.dma("sp", kvu.a[i * 128:(i + 1) * 128, :, :], O3[:], reads=[O3], writes=[kvu_s], final=True)
            if stop == 5:
                kb.finish(); return nc
            kb.op("pool", lambda e: e.tensor_copy(out=Qn[:, 1, :], in_=O3[:, 0, :]), reads=[O3], writes=[Qn])
            if sub == 1:
                kb.finish(); return nc
            kb.op("pe", lambda e: e.transpose(out=tq[:, 0, :], in_=Qn[:, 0, :], identity=identb[:]),
                  reads=[Qn, identb], writes=[tq])
            kb.op("pe", lambda e: e.transpose(out=tq[:, 1, :], in_=Qn[:, 1, :], identity=identb[:]),
                  reads=[Qn, identb], writes=[tq])
            if sub == 2:
                kb.finish(); return nc
            kb.op("act", lambda e: e.copy(out=QT[:, i * 128:(i + 1) * 128], in_=tq[:, 0, :]), reads=[tq], writes=[QT])
            if sub == 3:
                kb.finish(); return nc
            kb.op("act", lambda e: e.copy(out=KT[:, i * 128:(i + 1) * 128], in_=tq[:, 1, :]),
                  reads=[tq], writes=[KT])
            if stop == 6:
                kb.finish(); return nc
            if i < 64:
                kb.op("dve", lambda e: e.reduce_sum(out=ksum[:, i:i + 1], in_=KT[:, i * 128:(i + 1) * 128], axis=AX.X),
                      reads=[KT], writes=[ksum])
            if stop == 7:
                kb.finish(); return nc
        kv = ksum[:, :].rearrange("p (n t) -> p n t", t=2)
        kb.op("dve", lambda e: e.tensor_tensor(out=kmT[:], in0=kv[:, :, 0], in1=kv[:, :, 1], op=ALU.add),
              reads=[ksum], writes=[kmT])

        gps = kb.ps("gps", 4, 256, 32)
        sps = [kb.ps("sps%d" % i, i, 0, 256, split=2) for i in range(2)]
        ops = [kb.ps("ops%d" % i, 2 + i, 0, HD + 1) for i in range(2)]
        gpad = kb.sb("gpad", [128, 32], F32)
        m8 = kb.sb("m8", [128, 8], F32)
        sel = [kb.sb("sel%d" % i, [128, 32], F32) for i in range(2)]
        pT = [kb.sb("pT%d" % i, [128, 2, 128], BF16) for i in range(3)]
        acc = [kb.sb("acc%d" % i, [128, HD + 1], F32) for i in range(2)]
        ob = [kb.sb("ob%d" % i, [128, HD], F32) for i in range(2)]
        rc = [kb.sb("rc%d" % i, [128, 1], F32) for i in range(2)]
        kb.op("pool", lambda e: e.memset(gpad[:], NEG), writes=[gpad])
        it = 0
        for c in range(nchunks):
            own = c // 2
            qs = slice(c * 128, (c + 1) * 128)
            A, SEL, OB, RC = acc[c % 2], sel[c % 2], ob[c % 2], rc[c % 2]
            if own > 3:
                kb.op("pe", lambda e: e.matmul(gps[:], lhsT=QT[:, qs], rhs=kmT[:], start=True, stop=True),
                      reads=[QT, kmT], writes=[gps])
                kb.op("dve", lambda e: e.tensor_copy(out=gpad[:, 0:own], in_=gps[:, 0:own]), reads=[gps], writes=[gpad])
                w8 = max(own, 8)
                kb.op("dve", lambda e: e.max(out=m8[:], in_=gpad[:, 0:w8]), reads=[gpad], writes=[m8])
                kb.op("dve", lambda e: e.tensor_scalar(out=SEL[:, 0:own], in0=gpad[:, 0:own], scalar1=m8[:, 2:3],
                                                       scalar2=None, op0=ALU.is_ge), reads=[gpad, m8], writes=[SEL])
            for n in [own] + list(range(own)):
                if n == own:
                    kts = list(range(2 * own, c + 1))
                else:
                    kts = [2 * n, 2 * n + 1]
                SP, P, OP = sps[it % 2], pT[it % 3], ops[it % 2]
                it += 1
                for j, kt in enumerate(kts):
                    kb.op("pe", lambda e: e.matmul(SP[:, j, :], lhsT=KT[:, kt * 128:(kt + 1) * 128], rhs=QT[:, qs],
                                                   start=True, stop=True), reads=[KT, QT], writes=[SP])
                nj = len(kts)
                kb.op("act", lambda e: e.activation(out=P[:, 0:nj, :], in_=SP[:, 0:nj, :], func=AF.Exp, scale=SCALE),
                      reads=[SP], writes=[P])
                if n == own:
                    kb.op("pool", lambda e: e.tensor_tensor(out=P[:, nj - 1, :], in0=P[:, nj - 1, :], in1=trib[:],
                                                            op=ALU.mult), reads=[P, trib], writes=[P])
                for j, kt in enumerate(kts):
                    kb.op("pe", lambda e: e.matmul(OP[:], lhsT=P[:, j, :], rhs=Vx[:, kt, :],
                                                   start=(j == 0), stop=(j == nj - 1)), reads=[P, Vx], writes=[OP])
                if n == own:
                    kb.op("act", lambda e: e.copy(out=A[:], in_=OP[:]), reads=[OP], writes=[A])
                elif own > 3:
                    kb.op("dve", lambda e: e.scalar_tensor_tensor(out=A[:], in0=OP[:], scalar=SEL[:, n:n + 1], in1=A[:],
                                                                  op0=ALU.mult, op1=ALU.add),
                          reads=[OP, SEL, A], writes=[A])
                else:
                    kb.op("dve", lambda e: e.tensor_tensor(out=A[:], in0=OP[:], in1=A[:], op=ALU.add),
                          reads=[OP, A], writes=[A])
            kb.op("dve", lambda e: e.reciprocal(out=RC[:], in_=A[:, HD:HD + 1]), reads=[A], writes=[RC])
            kb.op("dve", lambda e: e.tensor_scalar(out=OB[:], in0=A[:, 0:HD], scalar1=RC[:, 0:1], scalar2=None,
                                                   op0=ALU.mult), reads=[A, RC], writes=[OB])
            kb.dma("sp", oo.a[c * 128:(c + 1) * 128, :], OB[:], reads=[OB], final=True)

        KcT = [kb.sb("KcT%d" % i, [128, NPAGES * 128], BF16) for i in range(2)]
        Vc = [kb.sb("Vc%d" % i, [128, NPAGES, HD + 1], BF16) for i in range(2)]
        vsb = [kb.sb("vsb%d" % i, [TS, HD + 1], BF16) for i in range(2)]
        tri4 = kb.sb("tri4", [TS, TS], F32)
        kmb = [kb.sb("kmb%d" % i, [128, 8], BF16) for i in range(2)]
        kmf = [kb.sb("kmf%d" % i, [128, 8], F32) for i in range(2)]
        g8 = [kb.sb("g8%d" % i, [TS, 8], F32) for i in range(2)]
        m8s = [kb.sb("m8s%d" % i, [TS, 8], F32) for i in range(2)]
        sels = [kb.sb("sels%d" % i, [TS, 8], F32) for i in range(2)]
        pTs = [kb.sb("pTs%d" % i, [128, NPAGES, TS], BF16) for i in range(2)]
        pn = [kb.sb("pn%d" % i, [TS, TS], BF16) for i in range(2)]
        pnf = [kb.sb("pnf%d" % i, [TS, TS], F32) for i in range(2)]
        accs = [kb.sb("accs%d" % i, [TS, HD + 1], F32) for i in range(2)]
        obs = [kb.sb("obs%d" % i, [TS, HD], F32) for i in range(2)]
        rcs = [kb.sb("rcs%d" % i, [TS, 1], F32) for i in range(2)]
        ssp = [kb.ps("ssp%d" % i, 5 + i, 0, 64, split=NPAGES) for i in range(2)]
        gsp = kb.ps("gsp", 7, 128, 8, parts=TS)
        snp = kb.ps("snp", 7, 136, TS, parts=TS)
        onp = kb.ps("onp", 7, 160, HD + 1, parts=TS)
        osp = [kb.ps("osp%d" % i, i, 0, HD + 1, parts=TS) for i in range(2)]
        for i in range(2):
            kb.op("pool", lambda e: e.memset(Vc[i][:, :, HD:HD + 1], 1.0), writes=[Vc[i]])
            kb.op("pool", lambda e: e.memset(vsb[i][:, HD:HD + 1], 1.0), writes=[vsb[i]])
        kb.op("pool", lambda e: e.tensor_copy(out=tri4[:], in_=trib[0:TS, 0:TS]), reads=[trib], writes=[tri4])
        it2 = 0
        for b in range(nseq):
            g, bl = b // 8, b % 8
            i2 = b % 2
            KC, VC, VS, KM, G8, M8, SL, PS_, PN, PNF, AS, OBS, RCS, SSP = (
                KcT[i2], Vc[i2], vsb[i2], kmb[i2], g8[i2], m8s[i2], sels[i2], pTs[i2], pn[i2], pnf[i2],
                accs[i2], obs[i2], rcs[i2], ssp[i2])
            cs = slice(S + b * TS, S + (b + 1) * TS)
            kb.dma("pool", KC[:, :].rearrange("d (j t) -> d j t", j=NPAGES),
                   scrK[g].a[bl * 16:(bl + 1) * 16, :].rearrange("j (d t) -> d j t", d=HD),
                   reads=[scrK[g]], writes=[KC])
            kb.dma("pool", VC[:, :, 0:HD],
                   scrV[g].a[bl * 16:(bl + 1) * 16, :].rearrange("j (t d) -> t j d", t=128),
                   reads=[scrV[g]], writes=[VC])
            kb.dma("pool", VS[:, 0:HD], kvu.a[S + b * TS:S + (b + 1) * TS, 1, :], reads=[kvu_s], writes=[VS])
            KMF = kmf[i2]
            kb.op("dve", lambda e: e.reduce_sum(out=KMF[:], in_=KC[:, :].rearrange("d (n t) -> d n t", n=8), axis=AX.X),
                  reads=[KC], writes=[KMF])
            kb.op("act", lambda e: e.copy(out=KM[:], in_=KMF[:]), reads=[KMF], writes=[KM])
            kb.op("pe", lambda e: e.matmul(gsp[:], lhsT=QT[:, cs], rhs=KM[:], start=True, stop=True),
                  reads=[QT, KM], writes=[gsp])
            kb.op("dve", lambda e: e.tensor_copy(out=G8[:], in_=gsp[:]), reads=[gsp], writes=[G8])
            kb.op("dve", lambda e: e.max(out=M8[:], in_=G8[:]), reads=[G8], writes=[M8])
            kb.op("dve", lambda e: e.tensor_scalar(out=SL[:], in0=G8[:], scalar1=M8[:, 2:3], scalar2=None,
                                                   op0=ALU.is_ge), reads=[G8, M8], writes=[SL])
            for j in range(NPAGES):
                kb.op("pe", lambda e: e.matmul(SSP[:, j, :], lhsT=KC[:, j * 128:(j + 1) * 128], rhs=QT[:, cs],
                                               start=True, stop=True), reads=[KC, QT], writes=[SSP])
            kb.op("act", lambda e: e.activation(out=PS_[:], in_=SSP[:], func=AF.Exp, scale=SCALE),
                  reads=[SSP], writes=[PS_])
            kb.op("pe", lambda e: e.matmul(snp[:], lhsT=KT[:, cs], rhs=QT[:, cs], start=True, stop=True),
                  reads=[KT, QT], writes=[snp])
            kb.op("act", lambda e: e.activation(out=PNF[:], in_=snp[:], func=AF.Exp, scale=SCALE),
                  reads=[snp], writes=[PNF])
            kb.op("dve", lambda e: e.tensor_tensor(out=PN[:], in0=PNF[:], in1=tri4[:], op=ALU.mult),
                  reads=[PNF, tri4], writes=[PN])
            kb.op("pe", lambda e: e.matmul(onp[:], lhsT=PN[:], rhs=VS[:], start=True, stop=True),
                  reads=[PN, VS], writes=[onp])
            kb.op("act", lambda e: e.copy(out=AS[:], in_=onp[:]), reads=[onp], writes=[AS])
            for n in range(8):
                OP = osp[it2 % 2]
                it2 += 1
                for jj in range(2):
                    j = 2 * n + jj
                    kb.op("pe", lambda e: e.matmul(OP[:], lhsT=PS_[:, j, :], rhs=VC[:, j, :],
                                                   start=(jj == 0), stop=(jj == 1)), reads=[PS_, VC], writes=[OP])
                kb.op("dve", lambda e: e.scalar_tensor_tensor(out=AS[:], in0=OP[:], scalar=SL[:, n:n + 1], in1=AS[:],
                                                              op0=ALU.mult, op1=ALU.add),
                      reads=[OP, SL, AS], writes=[AS])
            kb.op("dve", lambda e: e.reciprocal(out=RCS[:], in_=AS[:, HD:HD + 1]), reads=[AS], writes=[RCS])
            kb.op("dve", lambda e: e.tensor_scalar(out=OBS[:], in0=AS[:, 0:HD], scalar1=RCS[:, 0:1], scalar2=None,
                                                   op0=ALU.mult), reads=[AS, RCS], writes=[OBS])
            kb.dma("sp", oo.a[S + b * TS:S + (b + 1) * TS, :], OBS[:], reads=[OBS], final=True)
        kb.finish()
    return nc


NTB = 9
TB_ROWS = NTB * 128
NE = 16
DF = 768
TGS = [(0, 512), (512, 512), (1024, 128)]


def _barrier(kb, bufs):
    for e, eng in kb.E.items():
        for f in kb.E:
            if f != e and kb.cnt[f] > 0 and kb.seen[e].get(f, 0) < kb.cnt[f]:
                eng.wait_ge(kb.sem[f], kb.cnt[f])
                kb.seen[e][f] = kb.cnt[f]
        for b in bufs:
            if b.dsem is not None and kb.seen[e].get("d_" + b.name, 0) < b.dcnt:
                eng.wait_ge(b.dsem, b.dcnt)
                kb.seen[e]["d_" + b.name] = b.dcnt


def build_ffn(ne=NE, do_moe=True):
    nc = bass.Bass("TRN2", target_bir_lowering=False)
    es = ExitStack()
    with es:
        kb = KB(nc, es)
        kb.banks()
        xc = kb.dr("xc", [TB_ROWS, D], F32, "ExternalInput")
        oaT = kb.dr("oaT", [1024, TB_ROWS], F32, "ExternalInput")
        uTp = kb.dr("uTp", [1024, 15 + 1024], F32, "ExternalInput")
        uTs = kb.dr("uTs", [1024, 16, 19], F32, "ExternalInput")
        invc = kb.dr("invc", [1, 4 * 1024], F32, "ExternalInput")
        wpool = kb.dr("wpool", [4, 256, 256], F32, "ExternalInput")
        pscale = kb.dr("pscale", [128, 8], F32, "ExternalInput")
        wout = kb.dr("wout", [D, D], F32, "ExternalInput")
        gffn = kb.dr("gffn", [1, D], F32, "ExternalInput")
        wr = kb.dr("wr", [D, 20], F32, "ExternalInput")
        br = kb.dr("br", [1, 20], F32, "ExternalInput")
        wgate = kb.dr("wgate", [NE, D, DF], F32, "ExternalInput")
        wup = kb.dr("wup", [NE, D, DF], F32, "ExternalInput")
        wdown = kb.dr("wdown", [NE, DF, D], F32, "ExternalInput")
        y = kb.dr("y", [TB_ROWS, D], F32, "ExternalOutput")
        spool = kb.dr("spool", [176, 1024], F32, "ExternalInput")
        pso = kb.dr("pso", [176, 1024], F32, "ExternalOutput")
        allb = []

        def sb(name, shape, dt, st=None):
            t = TB((st or es).enter_context(nc.sbuf_tensor(name, shape, dt)), name)
            allb.append(t)
            return t

        onesf, identf, identb, trib = _consts(kb)
        allb.extend([onesf, identf, identb, trib])
        Y = sb("Y", [128, NTB, D], F32)
        comb = sb("comb", [128, NTB, NE], F32)
        spt = sb("spt", [128, 2, 1024], F32)
        kb.dma("sp", spt[:, 0, :], spool.a[0:128, :], writes=[spt])
        kb.dma("sp", spt[0:48, 1, :], spool.a[128:176, :], writes=[spt])
        kb.dma("sp", pso.a[0:128, :], spt[:, 0, :], reads=[spt], final=True)
        kb.dma("sp", pso.a[128:176, :], spt[0:48, 1, :], reads=[spt], final=True)
        with ExitStack() as esM:
            mixT = sb("mixT", [128, 16, TB_ROWS], BF16, esM)
            kb.dma("pool", mixT[:, 0:8, :], oaT.a.rearrange("(c p) t -> p c t", p=128), writes=[mixT])
            with ExitStack() as es1:
                dT = sb("dT", [128, 8, TB_ROWS], BF16, es1)
                ivc = sb("ivc", [128, 4, 1024], F32, es1)
                wp = sb("wp", [128, 8, 256], BF16, es1)
                psc = sb("psc", [128, 8], F32, es1)
                Up = [sb("Up%d" % i, [128, 1039], F32, es1) for i in range(2)]
                Ua = [sb("Ua%d" % i, [128, 1039], F32, es1) for i in range(2)]
                Us = [sb("Us%d" % i, [128, 16, 19], F32, es1) for i in range(2)]
                Usa = [sb("Usa%d" % i, [128, 16, 19], F32, es1) for i in range(2)]
                dtmp = sb("dtmp", [128, 1024], F32, es1)
                dtmps = sb("dtmps", [128, 16, 4], F32, es1)
                kb.op("pool", lambda e: e.memset(dT[:], 0.0), writes=[dT])
                kb.dma("sp", ivc[:, :, :].rearrange("p g t -> p (g t)"), invc.a[0, :].partition_broadcast(128), writes=[ivc])
                kb.dma("pool", wp[:], wpool.a.rearrange("g (c p) e -> p (g c) e", p=128), writes=[wp])
                kb.dma("sp", psc[:], pscale.a[:, :], writes=[psc])
                for j in range(8):
                    g = j // 2
                    U, UsJ = Up[j % 2], Us[j % 2]
                    kb.dma("sp", U[:], uTp.a[j * 128:(j + 1) * 128, :], writes=[U])
                    kb.dma("sp", UsJ[:], uTs.a[j * 128:(j + 1) * 128, :, :], writes=[UsJ])
                    cur, curs = U, UsJ
                    lo = 0
                    for k in range(g + 1):
                        sh = 1 << k
                        nxt, nxts = Ua[k % 2], Usa[k % 2]
                        l2 = lo + sh
                        kb.op("dve", lambda e: e.tensor_tensor(out=nxt[:, l2:1039], in0=cur[:, l2:1039],
                                                               in1=cur[:, lo:1039 - sh], op=ALU.add),
                              reads=[cur], writes=[nxt])
                        kb.op("pool", lambda e: e.tensor_tensor(out=nxts[:, :, l2:19], in0=curs[:, :, l2:19],
                                                                in1=curs[:, :, lo:19 - sh], op=ALU.add),
                              reads=[curs], writes=[nxts])
                        cur, curs = nxt, nxts
                        lo = l2
                    w = float(1 << (g + 1))
                    kb.op("dve", lambda e: e.tensor_tensor(out=dtmp[:], in0=cur[:, 15:1039], in1=ivc[:, g, :], op=ALU.mult),
                          reads=[cur, ivc], writes=[dtmp])
                    kb.op("dve", lambda e: e.tensor_tensor(out=dT[:, j, 0:1024], in0=dtmp[:], in1=U[:, 15:1039],
                                                           op=ALU.subtract), reads=[dtmp, U], writes=[dT])
                    kb.op("pool", lambda e: e.tensor_scalar(out=dtmps[:], in0=curs[:, :, 15:19], scalar1=1.0 / w,
                                                            scalar2=None, op0=ALU.mult), reads=[curs], writes=[dtmps])
                    kb.op("pool", lambda e: e.tensor_tensor(
                        out=dT[:, j, 1024:1088].rearrange("p (b t) -> p b t", t=4), in0=dtmps[:],
                        in1=UsJ[:, :, 15:19], op=ALU.subtract), reads=[dtmps, UsJ], writes=[dT])
                pp = [kb.ps("pp%d" % i, i, 0, 512) for i in range(2)]
                it = 0
                for je in range(8):
                    g, el = je // 2, je % 2
                    for (t0, tn) in TGS:
                        P = pp[it % 2]
                        it += 1
                        for cc in range(2):
                            kb.op("pe", lambda e: e.matmul(P[:, 0:tn], lhsT=wp[:, g * 2 + cc, el * 128:(el + 1) * 128],
                                                           rhs=dT[:, g * 2 + cc, t0:t0 + tn], start=(cc == 0), stop=(cc == 1)),
                                  reads=[wp, dT], writes=[P])
                        kb.op("act", lambda e: e.activation(out=mixT[:, 8 + je, t0:t0 + tn], in_=P[:, 0:tn], func=AF.Copy,
                                                            scale=psc[:, je:je + 1]), reads=[P, psc], writes=[mixT])
                _barrier(kb, allb)
            with ExitStack() as es2:
                wo = [sb("wo%d" % i, [128, 16, 512], BF16, es2) for i in range(2)]
                xs = [sb("xs%d" % i, [128, 512], F32, es2) for i in range(3)]
                zp = [kb.ps("zq%d" % i, 2 + i, 0, 512) for i in range(2)]
                it = 0
                for n in range(4):
                    W = wo[n % 2]
                    kb.dma("pool", W[:], wout.a[:, n * 512:(n + 1) * 512].rearrange("(c p) n -> p c n", p=128), writes=[W])
                    for i in range(NTB):
                        X, Z = xs[it % 3], zp[it % 2]
                        it += 1
                        kb.dma("sp", X[:], xc.a[i * 128:(i + 1) * 128, n * 512:(n + 1) * 512], writes=[X])
                        for c in range(16):
                            kb.op("pe", lambda e: e.matmul(Z[:], lhsT=mixT[:, c, i * 128:(i + 1) * 128], rhs=W[:, c, :],
                                                           start=(c == 0), stop=(c == 15)), reads=[mixT, W], writes=[Z])
                        kb.op("dve", lambda e: e.tensor_tensor(out=Y[:, i, n * 512:(n + 1) * 512], in0=Z[:], in1=X[:],
                                                               op=ALU.add), reads=[Z, X], writes=[Y])
                _barrier(kb, allb)
        h2T = sb("h2T", [128, 16, TB_ROWS], BF16)
        with ExitStack() as es3:
            gf = sb("gf", [128, D], F32, es3)
            brb = sb("brb", [128, 20], F32, es3)
            wrf = sb("wrf", [128, 16, 20], F32, es3)
            whi = sb("whi", [128, 16, 20], BF16, es3)
            wlo = sb("wlo", [128, 16, 20], BF16, es3)
            junk = sb("junk2", [128, D], BF16, es3)
            h2f = sb("h2f", [128, D], F32, es3)
            hhi = sb("hhi", [128, D], BF16, es3)
            hlo = sb("hlo", [128, D], BF16, es3)
            loT = sb("loT", [128, 16, 128], BF16, es3)
            st = sb("st3", [128, 8], F32, es3)
            lg = sb("lg", [128, 20], F32, es3)
            r_ = sb("rt", [128, 48], F32, es3)
            tpa = [kb.ps("tpa%d" % i, 4 + i, 0, 512, BF16, split=8) for i in range(2)]
            lgp = kb.ps("lgp", 6, 0, 20)
            kb.dma("sp", gf[:], gffn.a[0, :].partition_broadcast(128), writes=[gf])
            kb.dma("sp", brb[:], br.a[0, :].partition_broadcast(128), writes=[brb])
            kb.dma("sp", wrf[:], wr.a.rearrange("(c p) n -> p c n", p=128), writes=[wrf])
            kb.op("act", lambda e: e.copy(out=whi[:], in_=wrf[:]), reads=[wrf], writes=[whi])
            kb.op("dve", lambda e: e.tensor_tensor(out=wlo[:], in0=wrf[:], in1=whi[:], op=ALU.subtract),
                  reads=[wrf, whi], writes=[wlo])
            for i in range(NTB):
                ts = slice(i * 128, (i + 1) * 128)
                kb.op("act", lambda e: e.activation(out=junk[:], in_=Y[:, i, :], func=AF.Square, accum_out=st[:, 0:1]),
                      reads=[Y], writes=[junk, st])
                kb.op("act", lambda e: e.activation(out=st[:, 1:2], in_=st[:, 0:1], func=AF.Sqrt, scale=1.0 / D, bias=EPS),
                      reads=[st], writes=[st])
                kb.op("dve", lambda e: e.reciprocal(out=st[:, 2:3], in_=st[:, 1:2]), reads=[st], writes=[st])
                kb.op("dve", lambda e: e.scalar_tensor_tensor(out=h2f[:], in0=Y[:, i, :], scalar=st[:, 2:3], in1=gf[:],
                                                              op0=ALU.mult, op1=ALU.mult), reads=[Y, st, gf], writes=[h2f])
                kb.op("act", lambda e: e.copy(out=hhi[:], in_=h2f[:]), reads=[h2f], writes=[hhi])
                kb.op("dve", lambda e: e.tensor_tensor(out=hlo[:], in0=h2f[:], in1=hhi[:], op=ALU.subtract),
                      reads=[h2f, hhi], writes=[hlo])
                for (src, dst, off) in ((hhi, h2T, i * 128), (hlo, loT, 0)):
                    for half in range(2):
                        T = tpa[half]
                        for cc in range(8):
                            c = half * 8 + cc
                            kb.op("pe", lambda e: e.transpose(out=T[:, cc, :], in_=src[:, c * 128:(c + 1) * 128],
                                                              identity=identb[:]), reads=[src, identb], writes=[T])
                        kb.op("act", lambda e: e.copy(out=dst[:, half * 8:(half + 1) * 8, off:off + 128], in_=T[:]),
                              reads=[T], writes=[dst])
                k = 0
                for c in range(16):
                    for (a, b) in ((h2T[:, c, ts], whi), (h2T[:, c, ts], wlo), (loT[:, c, :], whi)):
                        kb.op("pe", lambda e: e.matmul(lgp[:], lhsT=a, rhs=b[:, c, :], start=(k == 0), stop=(k == 47)),
                              reads=[h2T, loT, whi, wlo], writes=[lgp])
                        k += 1
                kb.op("dve", lambda e: e.tensor_tensor(out=lg[:], in0=lgp[:], in1=brb[:], op=ALU.add),
                      reads=[lgp, brb], writes=[lg])
                R = r_
                gl = lg[:, 0:4]
                el3 = lg[:, 4:20].rearrange("p (g e) -> p g e", g=4)
                def dv(fn, reads, writes):
                    kb.op("dve", fn, reads=reads, writes=writes)
                dv(lambda e: e.reduce_max(out=R[:, 0:1], in_=gl, axis=AX.X), [lg], [R])
                dv(lambda e: e.tensor_scalar(out=R[:, 4:8], in0=gl, scalar1=R[:, 0:1], scalar2=None, op0=ALU.subtract), [lg, R], [R])
                kb.op("act", lambda e: e.activation(out=R[:, 8:12], in_=R[:, 4:8], func=AF.Exp, accum_out=R[:, 1:2]),
                      reads=[R], writes=[R])
                dv(lambda e: e.reciprocal(out=R[:, 2:3], in_=R[:, 1:2]), [R], [R])
                dv(lambda e: e.tensor_scalar(out=R[:, 12:16], in0=gl, scalar1=R[:, 0:1], scalar2=None, op0=ALU.is_ge), [lg, R], [R])
                dv(lambda e: e.tensor_tensor(out=R[:, 16:32].rearrange("p (g e) -> p g e", g=4), in0=el3,
                                             in1=R[:, 12:16].unsqueeze(2).to_broadcast([128, 4, 4]), op=ALU.mult), [lg, R], [R])
                dv(lambda e: e.tensor_reduce(out=R[:, 32:36], in_=R[:, 16:32].rearrange("p (g e) -> p e g", g=4),
                                             axis=AX.X, op=ALU.add), [R], [R])
                dv(lambda e: e.reduce_max(out=R[:, 36:37], in_=R[:, 32:36], axis=AX.X), [R], [R])
                dv(lambda e: e.tensor_scalar(out=R[:, 40:44], in0=R[:, 32:36], scalar1=R[:, 36:37], scalar2=NEG,
                                             op0=ALU.is_ge, op1=ALU.mult), [R], [R])
                dv(lambda e: e.tensor_tensor(out=R[:, 40:44], in0=R[:, 40:44], in1=R[:, 32:36], op=ALU.add), [R], [R])
                dv(lambda e: e.reduce_max(out=R[:, 37:38], in_=R[:, 40:44], axis=AX.X), [R], [R])
                dv(lambda e: e.tensor_scalar(out=R[:, 44:48], in0=R[:, 32:36], scalar1=R[:, 37:38], scalar2=None,
                                             op0=ALU.is_ge), [R], [R])
                dv(lambda e: e.tensor_scalar(out=R[:, 40:44], in0=R[:, 32:36], scalar1=R[:, 36:37], scalar2=None,
                                             op0=ALU.subtract), [R], [R])
                kb.op("act", lambda e: e.activation(out=R[:, 40:44], in_=R[:, 40:44], func=AF.Exp), reads=[R], writes=[R])
                dv(lambda e: e.tensor_tensor(out=R[:, 40:44], in0=R[:, 40:44], in1=R[:, 44:48], op=ALU.mult), [R], [R])
                dv(lambda e: e.reduce_sum(out=R[:, 38:39], in_=R[:, 40:44], axis=AX.X), [R], [R])
                dv(lambda e: e.reciprocal(out=R[:, 39:40], in_=R[:, 38:39]), [R], [R])
                dv(lambda e: e.tensor_scalar(out=R[:, 40:44], in0=R[:, 40:44], scalar1=R[:, 39:40], scalar2=R[:, 2:3],
                                             op0=ALU.mult, op1=ALU.mult), [R], [R])
                dv(lambda e: e.tensor_tensor(out=comb[:, i, :].rearrange("p (g e) -> p g e", g=4),
                                             in0=R[:, 12:16].unsqueeze(2).to_broadcast([128, 4, 4]),
                                             in1=R[:, 40:44].unsqueeze(1).to_broadcast([128, 4, 4]), op=ALU.mult), [R], [comb])
            _barrier(kb, allb)
        wg = sb("wg", [128, 16, DF], BF16)
        wu = sb("wu", [128, 16, DF], BF16)
        wd = sb("wd", [128, 6, D], BF16)
        actT = [sb("actT%d" % i, [128, 6, 512], BF16) for i in range(2)]
        sA = [sb("sA%d" % i, [128, 512], BF16) for i in range(2)]
        pa = [kb.ps("pa%d" % i, i, 0, 512) for i in range(2)]
        pb = [kb.ps("pb%d" % i, 2 + i, 0, 512) for i in range(2)]
        pd = [kb.ps("pd%d" % i, 4 + i, 0, 512) for i in range(2)]
        ia = idn = 0
        for ex in range(ne if do_moe else 0):
            kb.dma("pool", wg[:], wgate.a[ex].rearrange("(c p) f -> p c f", p=128), writes=[wg])
            kb.dma("pool", wu[:], wup.a[ex].rearrange("(c p) f -> p c f", p=128), writes=[wu])
            kb.dma("pool", wd[:], wdown.a[ex].rearrange("(c p) n -> p c n", p=128), writes=[wd])
            for gi, (t0, tn) in enumerate(TGS):
                AT = actT[gi % 2]
                for fc in range(6):
                    PA, PB_, SA = pa[ia % 2], pb[ia % 2], sA[ia % 2]
                    ia += 1
                    for c in range(16):
                        kb.op("pe", lambda e: e.matmul(PA[:, 0:tn], lhsT=wg[:, c, fc * 128:(fc + 1) * 128],
                                                       rhs=h2T[:, c, t0:t0 + tn], start=(c == 0), stop=(c == 15)),
                              reads=[wg, h2T], writes=[PA])
                    for c in range(16):
                        kb.op("pe", lambda e: e.matmul(PB_[:, 0:tn], lhsT=wu[:, c, fc * 128:(fc + 1) * 128],
                                                       rhs=h2T[:, c, t0:t0 + tn], start=(c == 0), stop=(c == 15)),
                              reads=[wu, h2T], writes=[PB_])
                    kb.op("act", lambda e: e.activation(out=SA[:, 0:tn], in_=PA[:, 0:tn], func=AF.Silu),
                          reads=[PA], writes=[SA])
                    kb.op("dve", lambda e: e.tensor_tensor(out=AT[:, fc, 0:tn], in0=PB_[:, 0:tn], in1=SA[:, 0:tn],
                                                           op=ALU.mult), reads=[PB_, SA], writes=[AT])
                for ii in range(tn // 128):
                    i = t0 // 128 + ii
                    for n in range(4):
                        PD = pd[idn % 2]
                        idn += 1
                        for fc in range(6):
                            kb.op("pe", lambda e: e.matmul(PD[:], lhsT=AT[:, fc, ii * 128:(ii + 1) * 128],
                                                           rhs=wd[:, fc, n * 512:(n + 1) * 512], start=(fc == 0), stop=(fc == 5)),
                                  reads=[AT, wd], writes=[PD])
                        kb.op("dve", lambda e: e.scalar_tensor_tensor(
                            out=Y[:, i, n * 512:(n + 1) * 512], in0=PD[:], scalar=comb[:, i, ex:ex + 1],
                            in1=Y[:, i, n * 512:(n + 1) * 512], op0=ALU.mult, op1=ALU.add),
                            reads=[PD, comb, Y], writes=[Y])
        for i in range(NTB):
            kb.dma("sp", y.a[i * 128:(i + 1) * 128, :], Y[:, i, :], reads=[Y], final=True)
        kb.finish()
    return nc


def run_attn(inputs):
    x = np.concatenate([np.asarray(inputs["x_prompt"], np.float32).reshape(S, D),
                        np.asarray(inputs["x_sample"], np.float32).reshape(NS, D)], 0)
    w_in = np.asarray(inputs["w_in"], np.float32)[0]
    ck = np.asarray(inputs["cache_k"], np.float32)[0]
    cv = np.asarray(inputs["cache_v"], np.float32)[0]
    pt = np.ascontiguousarray(np.asarray(inputs["page_table"], np.int32).reshape(-1, 1))
    gmix = np.asarray(inputs["g_mix"], np.float32).reshape(1, D)
    gq = np.asarray(inputs["g_q"], np.float32).reshape(1, HD)
    gk = np.asarray(inputs["g_k"], np.float32).reshape(1, HD)
    in_maps = []
    for h in range(NH):
        cols = np.concatenate([np.arange(j * 1024 + h * HD, j * 1024 + (h + 1) * HD) for j in range(4)])
        in_maps.append({
            "x": x, "wh": np.ascontiguousarray(w_in[:, cols]), "gmix": gmix, "gq": gq, "gk": gk,
            "kc": np.ascontiguousarray(ck[:, :, h, :].transpose(0, 2, 1)).reshape(NPHYS, HD * 128),
            "vc": np.ascontiguousarray(cv[:, :, h, :]).reshape(NPHYS, 128 * HD),
            "pt": pt,
        })
    nc = build_attn()
    res = run_bass_kernel_spmd(nc, in_maps, core_ids=list(range(8)))
    kvu = np.stack([np.asarray(r["kvu"]) for r in res.results], 0)
    oo = np.stack([np.asarray(r["oo"]) for r in res.results], 0)
    return kvu, oo


def run_ffn(inputs, kvu, oo):
    POOLW = (2, 4, 8, 16)
    xp = np.asarray(inputs["x_prompt"], np.float32).reshape(S, D)
    xs = np.asarray(inputs["x_sample"], np.float32).reshape(NS, D)
    sp = np.asarray(inputs["state_pool"], np.float32)[0]
    u = np.ascontiguousarray(kvu[:, :, 2, :].transpose(1, 0, 2)).reshape(NTOK, 1024)
    oa = np.ascontiguousarray(oo.transpose(1, 0, 2)).reshape(NTOK, 1024)
    wr = np.ascontiguousarray(np.concatenate([np.asarray(inputs["w_group_router"], np.float32)[0],
                                              np.asarray(inputs["w_expert_router"], np.float32)[0].reshape(D, 16)], 1))
    br = np.concatenate([np.asarray(inputs["b_group_router"], np.float32)[0].reshape(1, 4),
                         np.asarray(inputs["b_expert_router"], np.float32)[0].reshape(1, 16)], 1)
    common = {
        "wpool": np.asarray(inputs["w_pool"], np.float32)[0],
        "pscale": np.ascontiguousarray(np.asarray(inputs["pool_scale"], np.float32)[0].reshape(8, 128).T),
        "wout": np.asarray(inputs["w_out"], np.float32)[0],
        "gffn": np.asarray(inputs["g_ffn"], np.float32).reshape(1, D),
        "wr": wr, "br": np.ascontiguousarray(br),
        "wgate": np.asarray(inputs["w_gate"], np.float32)[0],
        "wup": np.asarray(inputs["w_up"], np.float32)[0],
        "wdown": np.asarray(inputs["w_down"], np.float32)[0],
    }
    in_maps = []
    for c in range(8):
        prow = np.arange(1024 * c, 1024 * (c + 1))
        srow = S + np.arange(64 * c, 64 * (c + 1))
        xc = np.zeros((TB_ROWS, D), np.float32)
        xc[:1024] = xp[prow]
        xc[1024:1088] = xs[64 * c:64 * (c + 1)]
        oaT = np.zeros((1024, TB_ROWS), np.float32)
        oaT[:, :1024] = oa[prow].T
        oaT[:, 1024:1088] = oa[srow].T
        uTp = np.zeros((1024, 1039), np.float32)
        uTp[:, 15:] = u[prow].T
        if c > 0:
            uTp[:, :15] = u[1024 * c - 15:1024 * c].T
        uTs = np.zeros((1024, 16, 19), np.float32)
        uTs[:, :, :15] = sp[16 * c:16 * (c + 1)].transpose(2, 0, 1)
        uTs[:, :, 15:] = u[srow].reshape(16, 4, 1024).transpose(2, 0, 1)
        pos = np.arange(1024 * c, 1024 * (c + 1))
        invc = np.stack([1.0 / np.minimum(float(w), pos + 1.0) for w in POOLW], 0).astype(np.float32).reshape(1, 4096)
        m = dict(common)
        m.update({"xc": xc, "oaT": oaT, "uTp": uTp, "uTs": uTs, "invc": invc,
                  "spool": np.ascontiguousarray(sp[16 * c:16 * (c + 1), 4:15, :]).reshape(176, 1024)})
        in_maps.append(m)
    nc = build_ffn()
    res = run_bass_kernel_spmd(nc, in_maps, core_ids=list(range(8)))
    ys = [np.asarray(r["y"]) for r in res.results]
    pso = [np.asarray(r["pso"]).reshape(16, 11, 1024) for r in res.results]
    return ys, pso, u


def kernel(**inputs):
    kvu, oo = run_attn(inputs)
    ys, pso, u = run_ffn(inputs, kvu, oo)
    f = np.float32
    y_prompt = np.concatenate([y[:1024] for y in ys], 0).reshape(1, S, D).astype(f)
    y_sample = np.concatenate([y[1024:1088] for y in ys], 0).reshape(NSEQ, TS, D).astype(f)
    k_all = np.ascontiguousarray(kvu[:, :, 0, :].transpose(1, 0, 2))
    v_all = np.ascontiguousarray(kvu[:, :, 1, :].transpose(1, 0, 2))
    k_prompt = k_all[:S].reshape(1, 1, S, NH, HD).astype(f)
    v_prompt = v_all[:S].reshape(1, 1, S, NH, HD).astype(f)
    k_sample = k_all[S:].reshape(1, NSEQ, TS, NH, HD).astype(f)
    v_sample = v_all[S:].reshape(1, NSEQ, TS, NH, HD).astype(f)
    pool_prompt = u[S - 15:S].reshape(1, 1, 15, 1024).astype(f)
    pool_sample = np.concatenate([np.concatenate(pso, 0), u[S:].reshape(NSEQ, TS, 1024)], 1).reshape(1, NSEQ, 15, 1024).astype(f)
    return (y_prompt, y_sample, k_prompt, v_prompt, pool_prompt, k_sample, v_sample, pool_sample)
```
